# Optimizing a Trainium2 kernel written in Bass

```python
import math
import jax, jax.numpy as jnp
from jax import lax
import numpy as np

D_MODEL = 1024
BATCH = 4
SEQ = 4096
DEPTH = 2

CHUNK = 64
QBLK = 128
ATT_HEADS = 16
ATT_HEAD_DIM = 64
ATT_WIDTH = ATT_HEADS * ATT_HEAD_DIM
POOL_WINDOWS = (2, 4, 8, 16)
POOL_GROUPS = len(POOL_WINDOWS)
POOL_WIDTH = D_MODEL
POOL_GROUP_DIM = POOL_WIDTH // POOL_GROUPS
SSM_EXPAND = 2
SSM_INNER = SSM_EXPAND * D_MODEL
SSM_HEAD_DIM = 64
SSM_HEADS = SSM_INNER // SSM_HEAD_DIM
SSM_GROUPS = 4
SSM_HEADS_PER_GROUP = SSM_HEADS // SSM_GROUPS
SSM_STATE = 128
SSM_CONV = 4
SSM_CONV_DIM = SSM_INNER + 2 * SSM_GROUPS * SSM_STATE
N_BRANCHES = 3
N_EXPERTS = 16
N_EXPERT_GROUPS = 4
EXPERTS_PER_GROUP = N_EXPERTS // N_EXPERT_GROUPS
TOP_K = 2
EXPERT_DFF = 512
MOE_BLOCK = 256
DEEPNORM_ALPHA = (2 * DEPTH) ** 0.25
DEEPNORM_BETA = (8 * DEPTH) ** -0.25
LN_EPS = 1e-5
RMS_EPS = 1e-6
IN_SPLITS = (ATT_WIDTH, ATT_WIDTH, ATT_WIDTH, ATT_HEADS, POOL_WIDTH,
             SSM_INNER, SSM_CONV_DIM, SSM_HEADS, N_BRANCHES * D_MODEL)
IN_WIDTH = sum(IN_SPLITS)

kernel_name = 'hybrid_fox_pool_ssd_grouped_moe_deepnorm'


def layer_norm(x, g=None, b=None):
    x32 = x.astype(jnp.float32)
    mu = jnp.mean(x32, axis=-1, keepdims=True)
    xc = x32 - mu
    y = xc * lax.rsqrt(jnp.mean(xc * xc, axis=-1, keepdims=True) + LN_EPS)
    if g is not None:
        y = y * g + b
    return y.astype(x.dtype)


def split_columns(proj):
    points, acc = [], 0
    for w in IN_SPLITS[:-1]:
        acc += w
        points.append(acc)
    return jnp.split(proj, points, axis=-1)


def forgetting_attention(q, k, v, log_f):
    S = q.shape[1]
    scale = ATT_HEAD_DIM ** -0.5
    F = jnp.transpose(jnp.cumsum(log_f, axis=1), (0, 2, 1))
    q, k, v = (jnp.transpose(t, (0, 2, 1, 3)) for t in (q, k, v))
    outs = []
    for blk in range(S // QBLK):
        s0, s1 = blk * QBLK, (blk + 1) * QBLK
        logits = jnp.einsum('bhqd,bhkd->bhqk', q[:, :, s0:s1], k[:, :, :s1]).astype(jnp.float32) * scale
        logits = logits + F[:, :, s0:s1, None] - F[:, :, None, :s1]
        mask = jnp.arange(s0, s1)[:, None] >= jnp.arange(s1)[None, :]
        p = jax.nn.softmax(jnp.where(mask, logits, -jnp.inf), axis=-1)
        outs.append(jnp.einsum('bhqk,bhkd->bhqd', p.astype(v.dtype), v[:, :, :s1]))
    o = jnp.concatenate(outs, axis=2)
    B_ = o.shape[0]
    return jnp.transpose(o, (0, 2, 1, 3)).reshape(B_, S, ATT_WIDTH)


def multiscale_pool(u, w_pool, pool_scale):
    Bsz, S, _ = u.shape
    u32 = u.astype(jnp.float32).reshape(Bsz, S, POOL_GROUPS, POOL_GROUP_DIM)
    cs = jnp.cumsum(u32, axis=1)
    pos = jnp.arange(1, S + 1, dtype=jnp.float32)[:, None]
    pooled = []
    for g, w in enumerate(POOL_WINDOWS):
        csg = cs[:, :, g]
        lagged = jnp.pad(csg, ((0, 0), (w, 0), (0, 0)))[:, :S]
        pooled.append((csg - lagged) / jnp.minimum(pos, float(w)) - u32[:, :, g])
    pooled = jnp.stack(pooled, axis=2)
    y = jnp.einsum('bsgc,gcd->bsgd', pooled, w_pool.astype(jnp.float32)).reshape(Bsz, S, POOL_WIDTH)
    return (y * pool_scale).astype(u.dtype)


def causal_depthwise_conv(u, w, b):
    out = lax.conv_general_dilated(
        u, w[:, None, :].astype(u.dtype), window_strides=(1,),
        padding=[(SSM_CONV - 1, 0)], dimension_numbers=('NWC', 'WIO', 'NWC'),
        feature_group_count=u.shape[-1])
    return out + b


def ssd_mixer(z, xbc, dt_raw, conv_w, conv_b, dt_bias, a_log, d_skip, norm_w, w_o):
    Bsz, S, _ = z.shape
    nc = S // CHUNK
    G, R, P, N, L = SSM_GROUPS, SSM_HEADS_PER_GROUP, SSM_HEAD_DIM, SSM_STATE, CHUNK
    f32 = jnp.float32
    xbc = jax.nn.silu(causal_depthwise_conv(xbc, conv_w, conv_b))
    xs, bm, cm = jnp.split(xbc, [SSM_INNER, SSM_INNER + G * N], axis=-1)
    xs = xs.astype(f32).reshape(Bsz, nc, L, G, R, P)
    bm = bm.astype(f32).reshape(Bsz, nc, L, G, N)
    cm = cm.astype(f32).reshape(Bsz, nc, L, G, N)
    dt = jax.nn.softplus(dt_raw.astype(f32) + dt_bias).reshape(Bsz, nc, L, G, R)
    a = -jnp.exp(a_log.astype(f32)).reshape(G, R)
    a_cs = jnp.cumsum(dt * a, axis=2)
    causal = jnp.tril(jnp.ones((L, L), bool))[:, :, None, None]
    seg = a_cs[:, :, :, None] - a_cs[:, :, None, :]
    decay = jnp.where(causal, jnp.exp(jnp.where(causal, seg, 0.0)), 0.0)
    cb = jnp.einsum('bclgn,bcsgn->bclsg', cm, bm)
    scores = cb[..., None] * decay * dt[:, :, None]
    y_diag = jnp.einsum('bclsgr,bcsgrp->bclgrp', scores, xs)
    decay_end = jnp.exp(a_cs[:, :, -1:] - a_cs)
    states = jnp.einsum('bcsgn,bcsgr,bcsgrp->bcgrpn', bm, decay_end * dt, xs)
    chunk_decay = jnp.exp(a_cs[:, :, -1])

    def step(h, inp):
        st, dec = inp
        return dec[..., None, None] * h + st, h

    h0 = jnp.zeros((Bsz, G, R, P, N), states.dtype)
    _, h_prev = lax.scan(step, h0, (jnp.moveaxis(states, 1, 0), jnp.moveaxis(chunk_decay, 1, 0)))
    h_prev = jnp.moveaxis(h_prev, 0, 1)
    y_off = jnp.einsum('bclgn,bcgrpn->bclgrp', cm, h_prev) * jnp.exp(a_cs)[..., None]
    y = y_diag + y_off + d_skip.astype(f32).reshape(G, R)[:, :, None] * xs
    y = y.reshape(Bsz, S, SSM_INNER) * jax.nn.silu(z.astype(f32))
    yg = y.reshape(Bsz, S, G, SSM_INNER // G)
    yg = yg * lax.rsqrt(jnp.mean(yg * yg, axis=-1, keepdims=True) + RMS_EPS)
    y = yg.reshape(Bsz, S, SSM_INNER) * norm_w
    return y.astype(z.dtype) @ w_o


def hybrid_mixer(h, w_in, b_forget, w_attn_o, w_pool, pool_scale, conv_w, conv_b,
                 dt_bias, a_log, d_skip, ssm_norm_w, w_ssm_o, w_out):
    Bsz, S, _ = h.shape
    proj = h @ w_in
    q, k, v, f_raw, u_pool, z, xbc, dt_raw, gate_raw = split_columns(proj)
    log_f = jax.nn.log_sigmoid(f_raw.astype(jnp.float32) + b_forget)
    hs = (Bsz, S, ATT_HEADS, ATT_HEAD_DIM)
    y_att = forgetting_attention(q.reshape(hs), k.reshape(hs), v.reshape(hs), log_f) @ w_attn_o
    y_pool = multiscale_pool(u_pool, w_pool, pool_scale)
    y_ssm = ssd_mixer(z, xbc, dt_raw, conv_w, conv_b, dt_bias, a_log, d_skip, ssm_norm_w, w_ssm_o)
    gates = jax.nn.sigmoid(gate_raw.astype(jnp.float32)).reshape(Bsz, S, N_BRANCHES, D_MODEL)
    merged = gates[:, :, 0] * y_att + gates[:, :, 1] * y_pool + gates[:, :, 2] * y_ssm
    return merged.astype(h.dtype) @ w_out


def grouped_moe(h, w_router, b_router, w1, w3, w2):
    Bsz, S, D = h.shape
    N = Bsz * S
    NK = N * TOP_K
    ht = h.reshape(N, D)
    probs = jax.nn.softmax((ht @ w_router).astype(jnp.float32), axis=-1)
    sel = probs + b_router
    group_score = lax.top_k(sel.reshape(N, N_EXPERT_GROUPS, EXPERTS_PER_GROUP), TOP_K)[0].sum(-1)
    g_idx = jnp.argmax(group_score, axis=-1)
    in_group = (jnp.arange(N_EXPERTS) // EXPERTS_PER_GROUP)[None, :] == g_idx[:, None]
    _, e_idx = lax.top_k(jnp.where(in_group, sel, -jnp.inf), TOP_K)
    gate_w = jnp.take_along_axis(probs, e_idx, axis=1)
    gate_w = gate_w / jnp.sum(gate_w, axis=-1, keepdims=True)
    e_flat = e_idx.reshape(NK)
    tok_flat = jnp.repeat(jnp.arange(N, dtype=jnp.int32), TOP_K)
    w_flat = gate_w.reshape(NK)
    order = jnp.argsort(e_flat)
    e_sorted, tok_sorted, w_sorted = e_flat[order], tok_flat[order], w_flat[order]
    counts = jnp.bincount(e_flat, length=N_EXPERTS)
    starts = jnp.cumsum(counts) - counts
    padded = ((counts + MOE_BLOCK - 1) // MOE_BLOCK) * MOE_BLOCK
    pends = jnp.cumsum(padded)
    pstarts = pends - padded
    dest = pstarts[e_sorted] + (jnp.arange(NK) - starts[e_sorted])
    P = NK + N_EXPERTS * MOE_BLOCK
    nblk = P // MOE_BLOCK
    slot_tok = jnp.full((P,), N, jnp.int32).at[dest].set(tok_sorted)
    slot_w = jnp.zeros((P,), jnp.float32).at[dest].set(w_sorted)
    blk_expert = jnp.minimum(
        jnp.searchsorted(pends, jnp.arange(nblk) * MOE_BLOCK, side='right'), N_EXPERTS - 1)
    x_pad = jnp.concatenate([ht, jnp.zeros((1, D), ht.dtype)], axis=0)
    xs = x_pad[slot_tok].reshape(nblk, MOE_BLOCK, D)

    def expert_block(args):
        xb, e = args
        return (jax.nn.silu(xb @ w1[e]) * (xb @ w3[e])) @ w2[e]

    ys = lax.map(expert_block, (xs, blk_expert)).reshape(P, D) * slot_w[:, None]
    out = jax.ops.segment_sum(ys, slot_tok, num_segments=N + 1)[:N]
    return out.reshape(Bsz, S, D).astype(h.dtype)


def setup_inputs(seed: int = 0) -> dict:
    key = jax.random.key(seed)
    ks = jax.random.split(key, 26)

    def nrm(k, shape, scale):
        return jax.random.normal(k, shape, jnp.float32) * scale

    col_scale = jnp.concatenate([
        jnp.ones((2 * ATT_WIDTH,), jnp.float32),
        jnp.full((ATT_WIDTH,), DEEPNORM_BETA, jnp.float32),
        jnp.ones((IN_WIDTH - 3 * ATT_WIDTH,), jnp.float32)])
    dt0 = jnp.exp(jax.random.uniform(ks[9], (DEPTH, SSM_HEADS), jnp.float32,
                                     minval=math.log(1e-3), maxval=math.log(1e-1)))
    return {
        'x': nrm(ks[0], (BATCH, SEQ, D_MODEL), 1.0),
        'c': nrm(ks[1], (BATCH, D_MODEL), 1.0),
        'w_in': nrm(ks[2], (DEPTH, D_MODEL, IN_WIDTH), D_MODEL ** -0.5) * col_scale,
        'b_forget': 4.0 + nrm(ks[3], (DEPTH, ATT_HEADS), 0.5),
        'w_attn_o': nrm(ks[4], (DEPTH, ATT_WIDTH, D_MODEL), DEEPNORM_BETA * ATT_WIDTH ** -0.5),
        'w_pool': nrm(ks[5], (DEPTH, POOL_GROUPS, POOL_GROUP_DIM, POOL_GROUP_DIM), DEEPNORM_BETA * POOL_GROUP_DIM ** -0.5),
        'pool_scale': 1.0 + nrm(ks[6], (DEPTH, POOL_WIDTH), 0.1),
        'conv_w': nrm(ks[7], (DEPTH, SSM_CONV, SSM_CONV_DIM), SSM_CONV ** -0.5),
        'conv_b': nrm(ks[8], (DEPTH, SSM_CONV_DIM), 0.02),
        'dt_bias': dt0 + jnp.log(-jnp.expm1(-dt0)),
        'a_log': jnp.log(jax.random.uniform(ks[10], (DEPTH, SSM_HEADS), jnp.float32, minval=1.0, maxval=16.0)),
        'd_skip': 1.0 + nrm(ks[11], (DEPTH, SSM_HEADS), 0.1),
        'ssm_norm_w': 1.0 + nrm(ks[12], (DEPTH, SSM_INNER), 0.1),
        'w_ssm_o': nrm(ks[13], (DEPTH, SSM_INNER, D_MODEL), DEEPNORM_BETA * SSM_INNER ** -0.5),
        'w_out': nrm(ks[14], (DEPTH, D_MODEL, D_MODEL), DEEPNORM_BETA * D_MODEL ** -0.5),
        'w_ada': nrm(ks[15], (DEPTH, D_MODEL, 6 * D_MODEL), D_MODEL ** -0.5),
        'b_ada': nrm(ks[16], (DEPTH, 6 * D_MODEL), 0.02),
        'ln_mix_g': 1.0 + nrm(ks[17], (DEPTH, D_MODEL), 0.1),
        'ln_mix_b': nrm(ks[18], (DEPTH, D_MODEL), 0.02),
        'ln_ffn_g': 1.0 + nrm(ks[19], (DEPTH, D_MODEL), 0.1),
        'ln_ffn_b': nrm(ks[20], (DEPTH, D_MODEL), 0.02),
        'w_router': nrm(ks[21], (D_MODEL, N_EXPERTS), D_MODEL ** -0.5),
        'b_router': nrm(ks[22], (N_EXPERTS,), 0.01),
        'w_exp_gate': nrm(ks[23], (DEPTH, N_EXPERTS, D_MODEL, EXPERT_DFF), DEEPNORM_BETA * D_MODEL ** -0.5),
        'w_exp_up': nrm(ks[24], (DEPTH, N_EXPERTS, D_MODEL, EXPERT_DFF), DEEPNORM_BETA * D_MODEL ** -0.5),
        'w_exp_down': nrm(ks[25], (DEPTH, N_EXPERTS, EXPERT_DFF, D_MODEL), DEEPNORM_BETA * EXPERT_DFF ** -0.5),
    }


def reference(x, c, w_in, b_forget, w_attn_o, w_pool, pool_scale, conv_w, conv_b,
              dt_bias, a_log, d_skip, ssm_norm_w, w_ssm_o, w_out, w_ada, b_ada,
              ln_mix_g, ln_mix_b, ln_ffn_g, ln_ffn_b, w_router, b_router,
              w_exp_gate, w_exp_up, w_exp_down):
    cond = jax.nn.silu(c)
    for l in range(DEPTH):
        mod = cond @ w_ada[l] + b_ada[l]
        sh_m, sc_m, g_m, sh_f, sc_f, g_f = jnp.split(mod[:, None, :], 6, axis=-1)
        h = layer_norm(x) * (1.0 + sc_m) + sh_m
        mix = hybrid_mixer(h, w_in[l], b_forget[l], w_attn_o[l], w_pool[l], pool_scale[l],
                           conv_w[l], conv_b[l], dt_bias[l], a_log[l], d_skip[l],
                           ssm_norm_w[l], w_ssm_o[l], w_out[l])
        x = layer_norm(DEEPNORM_ALPHA * x + g_m * mix, ln_mix_g[l], ln_mix_b[l])
        h = layer_norm(x) * (1.0 + sc_f) + sh_f
        ffn = grouped_moe(h, w_router, b_router, w_exp_gate[l], w_exp_up[l], w_exp_down[l])
        x = layer_norm(DEEPNORM_ALPHA * x + g_f * ffn, ln_ffn_g[l], ln_ffn_b[l])
    return x
```

```python
import contextlib
import numpy as np
import concourse.bass as bass
import concourse.mybir as mybir
from concourse.bass_utils import run_bass_kernel_spmd

F32 = mybir.dt.float32
BF16 = mybir.dt.bfloat16
AF = mybir.ActivationFunctionType
ALU = mybir.AluOpType

S = 4096
D = 1024
NTB = S // 128
KB = D // 128
DEPTH = 2
IN_W = 12336
C_Q, C_K, C_V, C_F, C_U, C_Z, C_X, C_DT, C_G = 0, 1024, 2048, 3072, 3088, 4112, 6160, 9232, 9264
ALPHA = (2 * DEPTH) ** 0.25
LN_EPS = 1e-5
RMS_EPS = 1e-6
NEG = -30000.0

ENGS = ("sync", "scalar", "vector", "gpsimd", "tensor")
NDMASEM = 32
NSWSEM = 12


class K:
    def __init__(self, nc):
        self.nc = nc
        self.es = contextlib.ExitStack()
        self.q = {e: [] for e in ENGS}
        self.cnt = {e: 0 for e in ENGS}
        self.sem = {e: self.es.enter_context(nc.semaphore("s_" + e)) for e in ENGS}
        self.dsem = [self.es.enter_context(nc.semaphore("d%d" % i)) for i in range(NDMASEM + NSWSEM)]
        self.dcnt = [0] * (NDMASEM + NSWSEM)
        self.dnext = 0
        self.dnext_sw = 0
        self.last_w = {}
        self.readers = {}
        self.seen = {e: {} for e in ENGS}
        self.uid = 0

    def sb(self, name, shape, dt, es=None):
        self.uid += 1
        return (es or self.es).enter_context(self.nc.sbuf_tensor("%s_%d" % (name, self.uid), list(shape), dt))

    def ps(self, name, shape, dt=F32, es=None):
        self.uid += 1
        return (es or self.es).enter_context(self.nc.psum_tensor("%s_%d" % (name, self.uid), list(shape), dt))

    def _deps(self, eng, reads, writes):
        deps = {}

        def add(tok):
            s, v = tok
            if deps.get(id(s), (None, 0))[1] < v:
                deps[id(s)] = (s, v)

        for k in reads:
            if k in self.last_w:
                add(self.last_w[k])
        for k in writes:
            if k in self.last_w:
                add(self.last_w[k])
            for tok in self.readers.get(k, {}).values():
                add(tok)
        waits = []
        seen = self.seen[eng]
        for sid, (s, v) in deps.items():
            if seen.get(sid, 0) >= v:
                continue
            seen[sid] = v
            waits.append((s, v))
        return waits

    def _commit(self, tok, reads, writes):
        for k in reads:
            self.readers.setdefault(k, {})[id(tok[0])] = tok
        for k in writes:
            self.last_w[k] = tok
            self.readers[k] = {}

    def op(self, eng, fn, reads=(), writes=()):
        waits = self._deps(eng, reads, writes)
        self.cnt[eng] += 1
        tok = (self.sem[eng], self.cnt[eng])
        self.q[eng].append((fn, waits, (self.sem[eng], 1)))
        self._commit(tok, reads, writes)

    def dma(self, eng, fn, reads=(), writes=()):
        waits = self._deps(eng, reads, writes)
        if eng == "gpsimd":
            j = NDMASEM + self.dnext_sw
            self.dnext_sw = (self.dnext_sw + 1) % NSWSEM
        else:
            j = self.dnext
            self.dnext = (self.dnext + 1) % NDMASEM
        s = self.dsem[j]
        if self.dcnt[j] > 0 and self.seen[eng].get(id(s), 0) < self.dcnt[j]:
            waits.append((s, self.dcnt[j]))
            self.seen[eng][id(s)] = self.dcnt[j]
        self.dcnt[j] += 16
        tok = (s, self.dcnt[j])
        self.q[eng].append((fn, waits, (s, 16)))
        self._commit(tok, reads, writes)

    def barrier(self):
        for e in ENGS:
            waits = []
            for f in ENGS:
                if f != e and self.cnt[f] > 0 and self.seen[e].get(id(self.sem[f]), 0) < self.cnt[f]:
                    waits.append((self.sem[f], self.cnt[f]))
                    self.seen[e][id(self.sem[f])] = self.cnt[f]
            for j in range(NDMASEM + NSWSEM):
                if self.dcnt[j] > 0 and self.seen[e].get(id(self.dsem[j]), 0) < self.dcnt[j]:
                    waits.append((self.dsem[j], self.dcnt[j]))
                    self.seen[e][id(self.dsem[j])] = self.dcnt[j]
            if self.cnt[e] > 0 and self.seen[e].get(id(self.sem[e]), 0) < self.cnt[e]:
                waits.append((self.sem[e], self.cnt[e]))
                self.seen[e][id(self.sem[e])] = self.cnt[e]
            if waits:
                self.q[e].append((None, waits, None))
        self.last_w = {}
        self.readers = {}

    def flush(self):
        nc = self.nc
        q = self.q
        if not any(q[e] for e in ENGS):
            return
        with nc.allow_non_contiguous_dma(reason="small strided parameter loads"):
            with nc.Block() as block:
                def mk(ename):
                    def body(e):
                        for fn, waits, inc in q[ename]:
                            for s, v in waits:
                                e.wait_ge(s, v)
                            if fn is not None:
                                fn(e).then_inc(inc[0], inc[1])
                    return body
                block.sync(mk("sync"))
                block.scalar(mk("scalar"))
                block.vector(mk("vector"))
                block.gpsimd(mk("gpsimd"))
                block.tensor(mk("tensor"))
        self.q = {e: [] for e in ENGS}

    @contextlib.contextmanager
    def stage(self):
        es = contextlib.ExitStack()
        try:
            yield es
            self.barrier()
            self.flush()
        finally:
            es.close()


def mm(e, out, lhsT, rhs, start, stop):
    return e.matmul(out, lhsT, rhs, start=start, stop=stop)


def load_bf16(k, dst, src, stg, dkey, skey, eng="gpsimd"):
    k.dma("sync", lambda e: e.dma_start(out=stg, in_=src), writes=[skey])
    k.op(eng, lambda e: e.tensor_copy(out=dst, in_=stg), reads=[skey], writes=[dkey])


class Ctx:
    def __init__(self, ext_in=(), ext_out=()):
        self.nc = bass.Bass("TRN2", target_bir_lowering=False)
        self.k = K(self.nc)
        self.ext_in = set(ext_in)
        self.ext_out = set(ext_out)
        self.dr = {}

    def dram(self, name, shape, dt=F32):
        if name not in self.dr:
            kind = "ExternalInput" if name in self.ext_in else ("ExternalOutput" if name in self.ext_out else "Internal")
            self.dr[name] = self.nc.dram_tensor(name, list(shape), dt, kind=kind).ap()
        return self.dr[name]

    def const(self, name, arr):
        if name not in self.dr:
            self.dr[name] = self.nc.inline_tensor(np.ascontiguousarray(arr), name=name).ap()
        return self.dr[name]


PARAM_SHAPES = {
    "w_in": [DEPTH, D, IN_W], "b_forget": [DEPTH, 16], "w_attn_o": [DEPTH, 1024, D], "w_pool": [DEPTH, 4, 256, 256],
    "pool_scale": [DEPTH, 1024], "conv_w": [DEPTH, 4, 3072], "conv_b": [DEPTH, 3072], "dt_bias": [DEPTH, 32],
    "a_log": [DEPTH, 32], "d_skip": [DEPTH, 32], "ssm_norm_w": [DEPTH, 2048], "w_ssm_o": [DEPTH, 2048, D],
    "w_out": [DEPTH, D, D], "w_ada": [DEPTH, D, 6 * D], "b_ada": [DEPTH, 6 * D], "ln_mix_g": [DEPTH, D],
    "ln_mix_b": [DEPTH, D], "ln_ffn_g": [DEPTH, D], "ln_ffn_b": [DEPTH, D], "w_router": [D, 16], "b_router": [16],
    "w_exp_gate": [DEPTH, 16, D, 512], "w_exp_up": [DEPTH, 16, D, 512], "w_exp_down": [DEPTH, 16, 512, D],
}


def setup_consts(cx):
    k = cx.k
    P = {}
    P["ident"] = k.sb("ident", [128, 128], F32)
    P["identb"] = k.sb("identb", [128, 128], BF16)
    P["ones"] = k.sb("ones", [128, 128], F32)
    idn = cx.const("c_ident", np.eye(128, dtype=np.float32))
    k.dma("sync", lambda e: e.dma_start(out=P["ident"][:], in_=idn), writes=["ident"])
    k.op("vector", lambda e: e.tensor_copy(out=P["identb"][:], in_=P["ident"][:]), reads=["ident"], writes=["identb"])
    k.op("vector", lambda e: e.memset(P["ones"][:], 1.0), writes=["ones"])
    P["modT"] = k.sb("modT", [128, 48], F32)
    P["gbc_m"] = k.sb("gbc_m", [128, D], F32)
    P["gbc_f"] = k.sb("gbc_f", [128, D], F32)
    P["lng"] = k.sb("lng", [128, D], F32)
    P["lnb"] = k.sb("lnb", [128, D], F32)
    P["acol"] = k.sb("acol", [64, 64, 32], F32)
    P["dtcol"] = k.sb("dtcol", [64, 64, 32], F32)
    return P


def stage_mod(cx, P, l):
    k = cx.k
    c = cx.dram("c", [D])
    w_ada = cx.dram("w_ada", PARAM_SHAPES["w_ada"])
    b_ada = cx.dram("b_ada", PARAM_SHAPES["b_ada"])
    with k.stage() as es:
        cT = k.sb("cT", [128, KB], F32, es)
        sg = k.sb("csg", [128, KB], F32, es)
        wt = [k.sb("wada%d" % i, [128, KB, 512], F32, es) for i in range(4)]
        brow = [k.sb("brow%d" % i, [1, 512], F32, es) for i in range(2)]
        row = [k.sb("row%d" % i, [1, 512], F32, es) for i in range(2)]
        prow = [k.ps("prow%d" % i, [1, 512], F32, es) for i in range(2)]
        pT = k.ps("pmodT", [128, 48], F32, es)
        pbc = [k.ps("pbc%d" % i, [128, 512], F32, es) for i in range(2)]
        k.dma("sync", lambda e: e.dma_start(out=cT[:], in_=c.rearrange("(kb p) -> p kb", p=128)), writes=["cT"])
        k.op("scalar", lambda e: e.activation(out=sg[:], in_=cT[:], func=AF.Sigmoid), reads=["cT"], writes=["csg"])
        k.op("vector", lambda e: e.tensor_tensor(out=cT[:], in0=cT[:], in1=sg[:], op=ALU.mult), reads=["cT", "csg"], writes=["cT"])
        for j in range(12):
            b = j % 2
            b4 = j % 4
            k.dma("sync", lambda e, j=j, b=b4: e.dma_start(out=wt[b][:], in_=w_ada[l, :, j * 512:(j + 1) * 512].rearrange("(kb p) n -> p kb n", p=128)),
                  writes=[("wada", b4)])
            k.dma("sync", lambda e, j=j, b=b: e.dma_start(out=brow[b][:], in_=b_ada[l, j * 512:(j + 1) * 512].unsqueeze(0)), writes=[("brow", b)])

            def f(e, b=b, b4=b4):
                for kb in range(KB):
                    ins = mm(e, prow[b][:], cT[:, kb:kb + 1], wt[b4][:, kb, :], kb == 0, kb == KB - 1)
                return ins
            k.op("tensor", f, reads=["cT", ("wada", b4)], writes=[("prow", b)])
            k.op("vector", lambda e, b=b: e.tensor_tensor(out=row[b][:], in0=prow[b][:], in1=brow[b][:], op=ALU.add),
                 reads=[("prow", b), ("brow", b)], writes=[("row", b)])

            def g(e, j=j, b=b):
                for i in range(4):
                    ins = mm(e, pT[:, j * 4 + i:j * 4 + i + 1], row[b][0:1, i * 128:(i + 1) * 128], P["ones"][0:1, 0:1], True, True)
                return ins
            k.op("tensor", g, reads=[("row", b), "ones"], writes=["pmodT"])
            if j in (4, 5, 10, 11):
                dst = P["gbc_m"] if j < 6 else P["gbc_f"]
                dkey = "gbc_m" if j < 6 else "gbc_f"
                half = j % 2
                k.op("tensor", lambda e, b=b: mm(e, pbc[b][:], P["ones"][0:1, 0:128], row[b][0:1, :], True, True),
                     reads=[("row", b), "ones"], writes=[("pbc", b)])
                k.op("vector", lambda e, b=b, dst=dst, half=half: e.tensor_copy(out=dst[:, half * 512:(half + 1) * 512], in_=pbc[b][:]),
                     reads=[("pbc", b)], writes=[dkey])
        k.op("vector", lambda e: e.tensor_copy(out=P["modT"][:], in_=pT[:]), reads=["pmodT"], writes=["modT"])
        k.op("vector", lambda e: e.tensor_scalar_add(out=P["modT"][:, 8:16], in0=P["modT"][:, 8:16], scalar1=1.0), reads=["modT"], writes=["modT"])
        k.op("vector", lambda e: e.tensor_scalar_add(out=P["modT"][:, 32:40], in0=P["modT"][:, 32:40], scalar1=1.0), reads=["modT"], writes=["modT"])


def emit_lnt(cx, P, es, x_src, hT, sh_col, sc_col, per_block=None):
    k = cx.k
    xt = [k.sb("lx%d" % i, [128, D], F32, es) for i in range(4)]
    xn = [k.sb("lxn%d" % i, [128, D], F32, es) for i in range(4)]
    st = [k.sb("lst%d" % i, [128, 2, 6], F32, es) for i in range(4)]
    mv = [k.sb("lmv%d" % i, [128, 4], F32, es) for i in range(4)]
    pst = [k.ps("lps%d" % i, [128, KB, 128], F32, es) for i in range(2)]
    h2f_tiles = [k.sb("h2f%d" % i, [128, KB, 128], F32, es) for i in range(2)] if per_block is not None else None

    def front(tb):
        b = tb % 2
        b4 = tb % 4
        k.dma("sync", lambda e, tb=tb, b=b4: e.dma_start(out=xt[b][:], in_=x_src[tb * 128:(tb + 1) * 128, :]), writes=[("lx", b4)])
        k.op("vector", lambda e, b=b4: e.bn_stats(out=st[b][:, 0, :], in_=xt[b][:, 0:512]), reads=[("lx", b4)], writes=[("lst", b4, 0)])
        k.op("vector", lambda e, b=b4: e.bn_stats(out=st[b][:, 1, :], in_=xt[b][:, 512:1024]), reads=[("lx", b4)], writes=[("lst", b4, 1)])
        k.op("vector", lambda e, b=b4: e.bn_aggr(out=mv[b][:, 0:2], in_=st[b][:]), reads=[("lst", b4, 0), ("lst", b4, 1)], writes=[("lmv", b4)])
        k.op("scalar", lambda e, b=b4: e.activation(out=mv[b][:, 2:3], in_=mv[b][:, 1:2], func=AF.Ln, bias=LN_EPS), reads=[("lmv", b4)], writes=[("lmv", b4)])
        k.op("scalar", lambda e, b=b4: e.activation(out=mv[b][:, 2:3], in_=mv[b][:, 2:3], func=AF.Exp, scale=-0.5), reads=[("lmv", b4)], writes=[("lmv", b4)])
        k.op("vector", lambda e, b=b4: e.tensor_scalar(out=xn[b][:], in0=xt[b][:], scalar1=mv[b][:, 0:1], scalar2=mv[b][:, 2:3], op0=ALU.subtract, op1=ALU.mult),
             reads=[("lx", b4), ("lmv", b4)], writes=[("lxn", b4)])

        def tr(e, b=b, b4=b4):
            for kb in range(KB):
                ins = e.transpose(out=pst[b][:, kb, :], in_=xn[b4][:, kb * 128:(kb + 1) * 128], identity=P["ident"][:])
            return ins
        k.op("tensor", tr, reads=[("lxn", b4), "ident"], writes=[("lps", b)])

    def back(tb):
        b = tb % 2
        b4 = tb % 4
        if per_block is None:
            for kb in range(KB):
                k.op("scalar", lambda e, b=b, kb=kb, tb=tb: e.activation(out=hT[:, kb, tb * 128:(tb + 1) * 128], in_=pst[b][:, kb, :], func=AF.Identity,
                                                                     scale=P["modT"][:, sc_col + kb:sc_col + kb + 1], bias=P["modT"][:, sh_col + kb:sh_col + kb + 1]),
                     reads=[("lps", b), "modT"], writes=[("hT", tb)])
        else:
            h2f = h2f_tiles[b]
            for kb in range(KB):
                k.op("vector", lambda e, b=b, kb=kb, h2f=h2f: e.tensor_scalar(out=h2f[:, kb, :], in0=pst[b][:, kb, :], scalar1=P["modT"][:, sc_col + kb:sc_col + kb + 1],
                                                                       scalar2=P["modT"][:, sh_col + kb:sh_col + kb + 1], op0=ALU.mult, op1=ALU.add),
                     reads=[("lps", b), "modT"], writes=[("h2f", b, kb)])
            k.op("scalar", lambda e, tb=tb, h2f=h2f: e.copy(out=hT[:, :, tb * 128:(tb + 1) * 128], in_=h2f[:]), reads=[("h2f", b, kb) for kb in range(KB)], writes=[("hT", tb)])
            return per_block(tb, [("h2f", b, kb) for kb in range(KB)], h2f)
        return None

    front(0)
    deferred = None
    for tb in range(NTB):
        if tb + 1 < NTB:
            front(tb + 1)
        nxt_def = back(tb)
        if deferred is not None:
            deferred()
        deferred = nxt_def
    if deferred is not None:
        deferred()

def stage_att(cx, P, hT, l):
    k = cx.k
    w_in = cx.dram("w_in", PARAM_SHAPES["w_in"])
    b_forget = cx.dram("b_forget", PARAM_SHAPES["b_forget"])
    attT_d = cx.dram("attT_d", [1024, S], BF16)
    um = np.zeros((128, 4, 512), np.float32)
    for r in range(4):
        um[:, r, :] = ((r * 128 + np.arange(128))[:, None] > np.arange(512)[None, :]).astype(np.float32)
    c_U = cx.const("c_U", um)
    sel = np.zeros((16, 2, 16, 66), np.float32)
    for h in range(16):
        sel[h, 0, h, 64] = 1.0
        sel[h, 1, h, 65] = 1.0
    c_sel = cx.const("c_sel", sel)
    hkeys = [("hT", tb) for tb in range(NTB)]
    with contextlib.ExitStack() as es:
        U = k.sb("U", [128, 4, 512], BF16, es)
        negI = k.sb("negI", [128, 128], BF16, es)
        selT = k.sb("selT", [16, 2, 16, 66], BF16, es)
        Fhi = k.sb("Fhi", [16, S], BF16, es)
        Flo = k.sb("Flo", [16, S], BF16, es)
        nFc = k.sb("nFc", [128, NTB, 16], F32, es)
        es1 = contextlib.ExitStack()
        wf = k.sb("wf", [128, KB, 16], BF16, es1)
        wfs = k.sb("wfs", [128, KB, 16], F32, es1)
        negb = k.sb("negb", [16, 1], F32, es1)
        NegF = k.sb("NegF", [16, S], F32, es1)
        pp = [k.ps("pp%d" % i, [128, 512], F32, es1) for i in range(2)]
        k.dma("gpsimd", lambda e: e.dma_start(out=U[:], in_=c_U), writes=["U"])
        k.dma("gpsimd", lambda e: e.dma_start(out=selT[:], in_=c_sel), writes=["selT"])
        k.op("vector", lambda e: e.tensor_scalar_mul(out=negI[:], in0=P["ident"][:], scalar1=NEG), reads=["ident"], writes=["negI"])
        load_bf16(k, wf[:], w_in[l, :, C_F:C_F + 16].rearrange("(kb p) n -> p kb n", p=128), wfs[:], "wf", "wfs")
        k.dma("sync", lambda e: e.dma_start(out=negb[:], in_=b_forget[l, :].unsqueeze(1)), writes=["negb"])
        k.op("vector", lambda e: e.tensor_scalar_mul(out=negb[:], in0=negb[:], scalar1=-1.0), reads=["negb"], writes=["negb"])
        for tc in range(8):
            b = tc % 2

            def f(e, tc=tc, b=b):
                for kb in range(KB):
                    ins = mm(e, pp[b][0:16, :], wf[:, kb, :], hT[:, kb, tc * 512:(tc + 1) * 512], kb == 0, kb == KB - 1)
                return ins
            k.op("tensor", f, reads=["wf"] + hkeys[tc * 4:tc * 4 + 4], writes=[("pp", b)])
            k.op("scalar", lambda e, tc=tc, b=b: e.activation(out=NegF[:, tc * 512:(tc + 1) * 512], in_=pp[b][0:16, :], func=AF.Exp, scale=-1.0, bias=negb[:, 0:1]),
                 reads=[("pp", b), "negb"], writes=["NegF"])
        k.op("scalar", lambda e: e.activation(out=NegF[:], in_=NegF[:], func=AF.Ln, bias=1.0), reads=["NegF"], writes=["NegF"])
        k.op("vector", lambda e: e.tensor_tensor_scan(out=NegF[:], data0=P["ones"][0:16, 0:1].to_broadcast([16, S]), data1=NegF[:], initial=0.0, op0=ALU.mult, op1=ALU.add),
             reads=["NegF", "ones"], writes=["NegF"])
        k.op("vector", lambda e: e.tensor_scalar_mul(out=Fhi[:], in0=NegF[:], scalar1=-1.0), reads=["NegF"], writes=["Fhi"])
        k.op("vector", lambda e: e.scalar_tensor_tensor(out=Flo[:], in0=NegF[:], scalar=-1.0, in1=Fhi[:], op0=ALU.mult, op1=ALU.subtract),
             reads=["NegF", "Fhi"], writes=["Flo"])

        def trf(e):
            for tb in range(NTB):
                ins = e.transpose(out=pp[0][:, tb * 16:(tb + 1) * 16], in_=NegF[0:16, tb * 128:(tb + 1) * 128], identity=P["ident"][0:16, 0:16])
            return ins
        k.op("tensor", trf, reads=["NegF", "ident"], writes=[("pp", 0)])
        k.op("vector", lambda e: e.tensor_copy(out=nFc[:].rearrange("p a b -> p (a b)"), in_=pp[0][:, 0:512]), reads=[("pp", 0)], writes=["nFc"])
        k.barrier()
        k.flush()
        es1.close()
        wqk = k.sb("wqk", [128, KB, 4, 128], BF16, es)
        wv = k.sb("wv", [128, KB, 256], BF16, es)
        qA = [k.sb("qA%d" % i, [66, S], BF16, es) for i in range(2)]
        kA = [k.sb("kA%d" % i, [66, S], BF16, es) for i in range(2)]
        vA = [k.sb("vA%d" % i, [128, NTB, 128], BF16, es) for i in range(2)]
        wst = k.sb("wst", [128, KB, 256], F32, es)
        pT = [k.sb("pT%d" % i, [128, 512], BF16, es) for i in range(4)]
        rr = k.sb("rr", [64, 512], F32, es)
        ot = [k.sb("ot%d" % i, [128, 512], BF16, es) for i in range(2)]
        ps = [k.ps("ps%d" % i, [128, 512], F32, es) for i in range(3)]
        po = [k.ps("po%d" % i, [128, 512], F32, es) for i in range(2)]
        pp = [k.ps("pp%d" % i, [128, 512], F32, es) for i in range(2)]
        pF = k.ps("pF", [2, 512], F32, es)
        for i in range(2):
            k.op("vector", lambda e, i=i: e.memset(vA[i][:], 1.0), writes=[("vA", i)])
        for i in range(2):
            k.op("vector", lambda e, i=i: e.memset(kA[i][64:66, :], 1.0), writes=[("kA", i)])
        cnt = {"pp": 0}

        def load_group(hg):
            k.dma("sync", lambda e, hg=hg: e.dma_start(out=wst[:], in_=wslice(w_in, l, C_Q + hg * 256, 256)), writes=["wst"])
            k.op("gpsimd", lambda e: e.tensor_copy(out=wqk[:, :, :, 0:64], in_=wst[:].rearrange("p kb (h n) -> p kb h n", n=64)), reads=["wst"], writes=["wqk"])
            k.dma("sync", lambda e, hg=hg: e.dma_start(out=wst[:], in_=wslice(w_in, l, C_K + hg * 256, 256)), reads=[], writes=["wst"])
            k.op("gpsimd", lambda e: e.tensor_copy(out=wqk[:, :, :, 64:128], in_=wst[:].rearrange("p kb (h n) -> p kb h n", n=64)), reads=["wst"], writes=["wqk"])
            load_bf16(k, wv[:], wslice(w_in, l, C_V + hg * 256, 256), wst[:], "wv", "wst")

        def proj_groups(h):
            hh = h % 4
            hb = h % 2
            out = []
            for tc in range(8):
                def gqk(tc=tc):
                    b = cnt["pp"] % 2
                    cnt["pp"] += 1

                    def fqk(e):
                        for kb in range(KB):
                            ins = mm(e, pp[b][:], wqk[:, kb, hh, :], hT[:, kb, tc * 512:(tc + 1) * 512], kb == 0, kb == KB - 1)
                        return ins
                    k.op("tensor", fqk, reads=["wqk"] + hkeys[tc * 4:tc * 4 + 4], writes=[("pp", b)])

                    def ff(e):
                        mm(e, pF[:], selT[:, 0, h, 64:66], Fhi[:, tc * 512:(tc + 1) * 512], True, False)
                        return mm(e, pF[:], selT[:, 1, h, 64:66], Flo[:, tc * 512:(tc + 1) * 512], False, True)
                    k.op("tensor", ff, reads=["selT", "Fhi", "Flo"], writes=["pF"])
                    k.op("vector", lambda e: e.tensor_scalar_mul(out=qA[hb][0:64, tc * 512:(tc + 1) * 512], in0=pp[b][0:64, :], scalar1=0.125),
                         reads=[("pp", b)], writes=[("qA", hb, tc)])
                    k.op("vector", lambda e: e.tensor_copy(out=kA[hb][0:64, tc * 512:(tc + 1) * 512], in_=pp[b][64:128, :]),
                         reads=[("pp", b), ("kA", hb)], writes=[("kAc", hb, tc)])
                    k.op("vector", lambda e: e.tensor_copy(out=qA[hb][64:66, tc * 512:(tc + 1) * 512], in_=pF[:]),
                         reads=["pF"], writes=[("qA2", hb, tc)])
                out.append(gqk)

                def gv(tc=tc):
                    b = cnt["pp"] % 2
                    cnt["pp"] += 1

                    def fv(e):
                        for t4 in range(4):
                            tb = tc * 4 + t4
                            for kb in range(KB):
                                ins = mm(e, pp[b][:, t4 * 64:(t4 + 1) * 64], hT[:, kb, tb * 128:(tb + 1) * 128], wv[:, kb, hh * 64:(hh + 1) * 64], kb == 0, kb == KB - 1)
                        return ins
                    k.op("tensor", fv, reads=["wv"] + hkeys[tc * 4:tc * 4 + 4], writes=[("pp", b)])
                    k.op("vector", lambda e: e.tensor_copy(out=vA[hb][:, tc * 4:(tc + 1) * 4, 64:128], in_=pp[b][:, 0:256].rearrange("p (t n) -> p t n", n=64)),
                         reads=[("pp", b), ("vA", hb)], writes=[("vAc", hb, tc)])
                out.append(gv)
            return out

        def finalize(h, tc):
            ob = tc % 2
            k.op("vector", lambda e: e.reciprocal(out=rr[:], in_=po[ob][0:64, :]), reads=[("po", ob)], writes=["rr"])
            k.op("vector", lambda e: e.tensor_tensor(out=ot[ob][64:128, :], in0=po[ob][64:128, :], in1=rr[:], op=ALU.mult), reads=[("po", ob), "rr"], writes=[("ot", ob)])
            k.dma("gpsimd", lambda e: e.dma_start(out=attT_d[h * 64:(h + 1) * 64, tc * 512:(tc + 1) * 512], in_=ot[ob][64:128, :]),
                  reads=[("ot", ob)], writes=[("attT_d", h, tc)])

        load_group(0)
        for g in proj_groups(0):
            g()
        its = [(tc, sb) for tc in range(8) for sb in range(4 * (tc + 1))]
        import os
        LOOK = 2
        DEFER = int(os.environ.get('ATT_DEFER', '3'))
        NOIL = int(os.environ.get('ATT_NOIL', '0'))
        gi = 0
        for h in range(16):
            hb = h % 2
            hh = h % 4
            nxt = proj_groups(h + 1) if h + 1 < 16 else []
            new_group = (h + 1 < 16) and ((h + 1) % 4 == 0)
            if new_group:
                load_group((h + 1) // 4)
            start_at = 48 if new_group else 12
            if NOIL:
                start_at = 10 ** 9
            step = 4 if new_group else 5
            pending = []
            for i in range(len(its) + LOOK):
                if i < len(its):
                    tc, sb = its[i]
                    sj = gi % 3
                    pj = gi % 4
                    gi += 1
                    diag = sb >= 4 * tc

                    def fs(e, tc=tc, sb=sb, sj=sj, hb=hb, diag=diag):
                        ins = mm(e, ps[sj][:], kA[hb][0:66, sb * 128:(sb + 1) * 128], qA[hb][0:66, tc * 512:(tc + 1) * 512], True, not diag)
                        if diag:
                            ins = mm(e, ps[sj][:], negI[:], U[:, sb - 4 * tc, :], False, True)
                        return ins
                    k.op("tensor", fs, reads=[("kA", hb), ("kAc", hb, sb // 4), ("qA", hb, tc), ("qA2", hb, tc), "negI", "U"], writes=[("ps", sj)])
                    k.op("scalar", lambda e, sb=sb, sj=sj, pj=pj, h=h: e.activation(out=pT[pj][:], in_=ps[sj][:], func=AF.Exp, bias=nFc[:, sb, h:h + 1], scale=1.0),
                         reads=[("ps", sj), "nFc"], writes=[("pT", pj)])
                    its_pj = pj
                    if i == 0:
                        pjs = []
                    pjs.append(pj)
                if i >= LOOK:
                    tc, sb = its[i - LOOK]
                    pj = pjs[i - LOOK]
                    ob = tc % 2
                    nsb = 4 * (tc + 1)
                    k.op("tensor", lambda e, sb=sb, pj=pj, ob=ob, hb=hb, nsb=nsb: mm(e, po[ob][:], vA[hb][:, sb, :], pT[pj][:], sb == 0, sb == nsb - 1),
                         reads=[("vA", hb), ("vAc", hb, sb // 4), ("pT", pj)], writes=[("po", ob)])
                    if sb == nsb - 1:
                        pending.append((i + DEFER, tc))
                if nxt and i >= start_at and (i - start_at) % step == 0:
                    nxt.pop(0)()
                while pending and (pending[0][0] <= i or i == len(its) + LOOK - 1):
                    finalize(h, pending.pop(0)[1])
            while nxt:
                nxt.pop(0)()
        k.barrier()
        k.flush()

def wslice(w_in, l, c0, n):
    return w_in[l, :, c0:c0 + n].rearrange("(kb p) n -> p kb n", p=128)


HKEYS = [("hT", tb) for tb in range(NTB)]


def stage_gates(cx, P, hT, l):
    k = cx.k
    w_in = cx.dram("w_in", PARAM_SHAPES["w_in"])
    gT_d = cx.dram("gT_d", [3072, S], BF16)
    with k.stage() as es:
        wg = [k.sb("wg%d" % i, [128, KB, 128], BF16, es) for i in range(2)]
        wgs = [k.sb("wgs%d" % i, [128, KB, 128], F32, es) for i in range(2)]
        stg = [k.sb("gst%d" % i, [128, S], BF16, es) for i in range(2)]
        pp = [k.ps("gpp%d" % i, [128, 512], F32, es) for i in range(2)]
        n = 0
        for cb in range(24):
            b = cb % 2
            load_bf16(k, wg[b][:], wslice(w_in, l, C_G + cb * 128, 128), wgs[b][:], ("wg", b), ("wgs", b))
            for tc in range(8):
                j = n % 2
                n += 1

                def f(e, b=b, tc=tc, j=j):
                    for kb in range(KB):
                        ins = mm(e, pp[j][:], wg[b][:, kb, :], hT[:, kb, tc * 512:(tc + 1) * 512], kb == 0, kb == KB - 1)
                    return ins
                k.op("tensor", f, reads=[("wg", b)] + HKEYS[tc * 4:tc * 4 + 4], writes=[("gpp", j)])
                k.op("scalar", lambda e, b=b, tc=tc, j=j: e.activation(out=stg[b][:, tc * 512:(tc + 1) * 512], in_=pp[j][:], func=AF.Sigmoid),
                     reads=[("gpp", j)], writes=[("gst", b)])
            k.dma("scalar", lambda e, cb=cb, b=b: e.dma_start(out=gT_d[cb * 128:(cb + 1) * 128, :], in_=stg[b][:]), reads=[("gst", b)], writes=[("gT_d", cb)])


def stage_z(cx, P, hT, l):
    k = cx.k
    w_in = cx.dram("w_in", PARAM_SHAPES["w_in"])
    zs_d = cx.dram("zs_d", [S, 2048], BF16)
    with k.stage() as es:
        wz = [k.sb("wz%d" % i, [128, KB, 512], BF16, es) for i in range(2)]
        wzs = [k.sb("wzs%d" % i, [128, KB, 512], F32, es) for i in range(2)]
        stg = [k.sb("zst%d" % i, [128, 512], BF16, es) for i in range(3)]
        pp = [k.ps("zpp%d" % i, [128, 512], F32, es) for i in range(2)]
        n = 0
        for cc in range(4):
            b = cc % 2
            k.dma("gpsimd", lambda e, cc=cc, b=b: e.dma_start(out=wz[b][:], in_=wslice(w_in, l, C_Z + cc * 512, 512)), writes=[("wz", b)])
            for tb in range(NTB):
                j = n % 2
                i = n % 3
                n += 1

                def f(e, b=b, tb=tb, j=j):
                    for kb in range(KB):
                        ins = mm(e, pp[j][:], hT[:, kb, tb * 128:(tb + 1) * 128], wz[b][:, kb, :], kb == 0, kb == KB - 1)
                    return ins
                k.op("tensor", f, reads=[("wz", b), ("hT", tb)], writes=[("zpp", j)])
                k.op("scalar", lambda e, j=j, i=i: e.activation(out=stg[i][:], in_=pp[j][:], func=AF.Silu), reads=[("zpp", j)], writes=[("zst", i)])
                k.dma("scalar", lambda e, cc=cc, tb=tb, i=i: e.dma_start(out=zs_d[tb * 128:(tb + 1) * 128, cc * 512:(cc + 1) * 512], in_=stg[i][:]),
                      reads=[("zst", i)], writes=[("zs_d", cc, tb)])


def stage_pool(cx, P, hT, l):
    k = cx.k
    w_in = cx.dram("w_in", PARAM_SHAPES["w_in"])
    w_pool = cx.dram("w_pool", PARAM_SHAPES["w_pool"])
    pool_scale = cx.dram("pool_scale", PARAM_SHAPES["pool_scale"])
    ypT_d = cx.dram("ypT_d", [1024, S], BF16)
    c_inv = cx.const("c_invpos", np.broadcast_to(1.0 / np.arange(1, 17, dtype=np.float32), (128, 16)))
    with k.stage() as es:
        wu = [k.sb("wu%d" % i, [128, KB, 128], BF16, es) for i in range(2)]
        wp = k.sb("wp", [128, 2, 256], BF16, es)
        wps = k.sb("wps", [128, 2, 256], F32, es)
        wus = [k.sb("wus%d" % i, [128, KB, 128], F32, es) for i in range(2)]
        psc = k.sb("psc", [128, 8], F32, es)
        invp = k.sb("invp", [128, 16], F32, es)
        A_ = [k.sb("pA%d" % i, [128, S], F32, es) for i in range(2)]
        T = [k.sb("pT%d" % i, [128, S], F32, es) for i in range(2)]
        tmp = k.sb("ptmp", [128, 16], F32, es)
        pooled = k.sb("pooled", [128, 2, S], BF16, es)
        stg = [k.sb("pst%d" % i, [128, S], BF16, es) for i in range(1)]
        pp = [k.ps("ppp%d" % i, [128, 512], F32, es) for i in range(2)]
        k.dma("sync", lambda e: e.dma_start(out=psc[:], in_=pool_scale[l, :].rearrange("(cb p) -> p cb", p=128)), writes=["psc"])
        k.dma("sync", lambda e: e.dma_start(out=invp[:], in_=c_inv), writes=["invp"])
        n = 0
        for g in range(4):
            w = 2 ** (g + 1)
            load_bf16(k, wp[:], w_pool[l, g].rearrange("(cb p) n -> p cb n", p=128), wps[:], "wp", "wps")
            for j in range(2):
                cb = g * 2 + j
                b = cb % 2
                A, akey = A_[b], ("pA", b)
                load_bf16(k, wu[b][:], wslice(w_in, l, C_U + cb * 128, 128), wus[b][:], ("wu", b), ("wus", b))
                for tc in range(8):
                    pj = n % 2
                    n += 1

                    def f(e, b=b, tc=tc, pj=pj):
                        for kb in range(KB):
                            ins = mm(e, pp[pj][:], wu[b][:, kb, :], hT[:, kb, tc * 512:(tc + 1) * 512], kb == 0, kb == KB - 1)
                        return ins
                    k.op("tensor", f, reads=[("wu", b)] + HKEYS[tc * 4:tc * 4 + 4], writes=[("ppp", pj)])
                    k.op("scalar", lambda e, tc=tc, pj=pj, A=A: e.copy(out=A[:, tc * 512:(tc + 1) * 512], in_=pp[pj][:]), reads=[("ppp", pj)], writes=[akey])
                src, skey = A, akey
                sh = 1
                ti = 0
                while sh < w:
                    dst, dkey = T[ti], ("pT", ti)
                    k.op("vector", lambda e, src=src, dst=dst, sh=sh: e.tensor_tensor(out=dst[:, sh:S], in0=src[:, sh:S], in1=src[:, 0:S - sh], op=ALU.add),
                         reads=[skey], writes=[dkey])
                    k.op("vector", lambda e, src=src, dst=dst, sh=sh: e.tensor_copy(out=dst[:, 0:sh], in_=src[:, 0:sh]), reads=[skey, dkey], writes=[dkey])
                    src, skey = dst, dkey
                    ti = 1 - ti
                    sh *= 2
                k.op("vector", lambda e, src=src, j=j, w=w, A=A: e.scalar_tensor_tensor(out=pooled[:, j, :], in0=src[:], scalar=1.0 / w, in1=A[:], op0=ALU.mult, op1=ALU.subtract),
                     reads=[skey, akey], writes=[("pooled", j)])
                k.op("vector", lambda e, src=src, w=w: e.tensor_tensor(out=tmp[:, 0:w], in0=src[:, 0:w], in1=invp[:, 0:w], op=ALU.mult), reads=[skey, "invp"], writes=["ptmp"])
                k.op("vector", lambda e, j=j, w=w, A=A: e.tensor_tensor(out=pooled[:, j, 0:w], in0=tmp[:, 0:w], in1=A[:, 0:w], op=ALU.subtract),
                     reads=["ptmp", akey, ("pooled", j)], writes=[("pooled", j)])
            for j2 in range(2):
                ob = g * 2 + j2
                sb_ = 0
                for tc in range(8):
                    pj = n % 2
                    n += 1

                    def f2(e, j2=j2, tc=tc, pj=pj):
                        for cbk in range(2):
                            ins = mm(e, pp[pj][:], wp[:, cbk, j2 * 128:(j2 + 1) * 128], pooled[:, cbk, tc * 512:(tc + 1) * 512], cbk == 0, cbk == 1)
                        return ins
                    k.op("tensor", f2, reads=["wp", ("pooled", 0), ("pooled", 1)], writes=[("ppp", pj)])
                    k.op("scalar", lambda e, ob=ob, sb_=sb_, tc=tc, pj=pj: e.activation(out=stg[sb_][:, tc * 512:(tc + 1) * 512], in_=pp[pj][:], func=AF.Copy, scale=psc[:, ob:ob + 1]),
                         reads=[("ppp", pj), "psc"], writes=[("pst", sb_)])
                k.dma("scalar", lambda e, ob=ob, sb_=sb_: e.dma_start(out=ypT_d[ob * 128:(ob + 1) * 128, :], in_=stg[sb_][:]), reads=[("pst", sb_)], writes=[("ypT_d", ob)])


def stage_ssdprep(cx, P, hT, l):
    k = cx.k
    w_in = cx.dram("w_in", PARAM_SHAPES["w_in"])
    conv_w = cx.dram("conv_w", PARAM_SHAPES["conv_w"])
    conv_b = cx.dram("conv_b", PARAM_SHAPES["conv_b"])
    dt_bias = cx.dram("dt_bias", PARAM_SHAPES["dt_bias"])
    a_log = cx.dram("a_log", PARAM_SHAPES["a_log"])
    xs_d = cx.dram("xs_d", [S, 2048], BF16)
    Btok_d = cx.dram("Btok_d", [S, 512], BF16)
    BT_d = cx.dram("BT_d", [512, S], BF16)
    CT_d = cx.dram("CT_d", [512, S], BF16)
    acs_d = cx.dram("acs_d", [32, S], F32)
    with k.stage() as es:
        wx = [k.sb("wx%d" % i, [128, KB, 128], BF16, es) for i in range(2)]
        wxs = [k.sb("wxs%d" % i, [128, KB, 128], F32, es) for i in range(2)]
        cw = [k.sb("cw%d" % i, [128, 5], F32, es) for i in range(2)]
        xT = [k.sb("xT%d" % i, [128, S + 4], F32, es) for i in range(2)]
        acc = [k.sb("cacc%d" % i, [128, S], F32, es) for i in range(2)]
        xa = [k.sb("xa%d" % i, [128, S], BF16, es) for i in range(2)]
        tst = [k.sb("tst%d" % i, [128, 8, 128], BF16, es) for i in range(2)]
        pp = [k.ps("spp%d" % i, [128, 512], F32, es) for i in range(2)]
        ptr = [k.ps("sptr%d" % i, [128, 8, 128], BF16, es) for i in range(2)]
        for i in range(2):
            k.op("vector", lambda e, i=i: e.memset(xT[i][:, 0:4], 0.0), writes=[("xT0", i)])
        n = 0
        nt = 0
        cntr = {"n": 0, "nt": 0}

        def sec_a(cb):
            b = cb % 2
            load_bf16(k, wx[b][:], wslice(w_in, l, C_X + cb * 128, 128), wxs[b][:], ("wx", b), ("wxs", b))
            k.dma("sync", lambda e: e.dma_start(out=cw[b][:, 0:4], in_=conv_w[l, :, cb * 128:(cb + 1) * 128].rearrange("j c -> c j")), writes=[("cw", b)])
            k.dma("sync", lambda e: e.dma_start(out=cw[b][:, 4:5], in_=conv_b[l, cb * 128:(cb + 1) * 128].unsqueeze(1)), writes=[("cwb", b)])
            for tc in range(8):
                pj = cntr["n"] % 2
                cntr["n"] += 1

                def f(e, tc=tc, pj=pj):
                    for kb in range(KB):
                        ins = mm(e, pp[pj][:], wx[b][:, kb, :], hT[:, kb, tc * 512:(tc + 1) * 512], kb == 0, kb == KB - 1)
                    return ins
                k.op("tensor", f, reads=[("wx", b)] + HKEYS[tc * 4:tc * 4 + 4], writes=[("spp", pj)])
                k.op("scalar", lambda e, tc=tc, pj=pj: e.copy(out=xT[b][:, 3 + tc * 512:3 + (tc + 1) * 512], in_=pp[pj][:]), reads=[("spp", pj)], writes=[("xT", b)])
            k.op("scalar", lambda e: e.activation(out=acc[b][:], in_=xT[b][:, 0:S], func=AF.Copy, scale=cw[b][:, 0:1]), reads=[("xT", b), ("xT0", b), ("cw", b)], writes=[("cacc", b)])

        def sec_b(cb):
            b = cb % 2
            for j in range(1, 4):
                k.op("vector", lambda e, j=j: e.scalar_tensor_tensor(out=acc[b][:], in0=xT[b][:, j:j + S], scalar=cw[b][:, j:j + 1], in1=acc[b][:], op0=ALU.mult, op1=ALU.add),
                     reads=[("xT", b), ("xT0", b), ("cw", b), ("cacc", b)], writes=[("cacc", b)])

        def sec_c(cb):
            b = cb % 2
            k.op("scalar", lambda e: e.activation(out=xa[b][:], in_=acc[b][:], func=AF.Silu, bias=cw[b][:, 4:5]), reads=[("cacc", b), ("cwb", b)], writes=[("xa", b)])

        def sec_d(cb):
            b = cb % 2
            if cb < 20:
                dst = xs_d[:, cb * 128:(cb + 1) * 128] if cb < 16 else Btok_d[:, (cb - 16) * 128:(cb - 15) * 128]
                for tg in range(4):
                    tj = cntr["nt"] % 2
                    cntr["nt"] += 1

                    def ftr(e, tg=tg, tj=tj):
                        for i in range(8):
                            tb = tg * 8 + i
                            ins = e.transpose(out=ptr[tj][:, i, :], in_=xa[b][:, tb * 128:(tb + 1) * 128], identity=P["identb"][:])
                        return ins
                    k.op("tensor", ftr, reads=[("xa", b), "identb"], writes=[("sptr", tj)])
                    k.op("vector", lambda e, tj=tj: e.tensor_copy(out=tst[tj][:], in_=ptr[tj][:]), reads=[("sptr", tj)], writes=[("tst", tj)])
                    k.dma("scalar", lambda e, tg=tg, tj=tj: e.dma_start(out=dst[tg * 1024:(tg + 1) * 1024, :].rearrange("(i p) c -> p i c", p=128), in_=tst[tj][:]),
                          reads=[("tst", tj)], writes=[("tok_d", cb, tg)])
            if cb >= 16:
                dT = BT_d if cb < 20 else CT_d
                r0 = (cb - 16) * 128 if cb < 20 else (cb - 20) * 128
                k.dma("scalar", lambda e: e.dma_start(out=dT[r0:r0 + 128, :], in_=xa[b][:]), reads=[("xa", b)], writes=[("fm_d", cb)])

        for i in range(26):
            if i < 24:
                sec_a(i)
            if i >= 2:
                sec_d(i - 2)
            if i < 24:
                sec_b(i)
            if 1 <= i <= 24:
                sec_c(i - 1)
    with k.stage() as es:
        wxs = [k.sb("wds", [128, KB, 32], F32, es)]
        wdt = k.sb("wdt", [128, KB, 32], BF16, es)
        dtb = k.sb("dtb", [32, 2], F32, es)
        dtT = k.sb("dtT", [32, S], F32, es)
        Gc = k.sb("Gc", [32, S], F32, es)
        Gprev = k.sb("Gprev", [32, 64], F32, es)
        pp = [k.ps("dpp%d" % i, [128, 512], F32, es) for i in range(2)]
        pc = k.ps("spc", [64, 16, 32], F32, es)
        n = 0
        load_bf16(k, wdt[:], wslice(w_in, l, C_DT, 32), wxs[0][:], "wdt", "wds")
        k.dma("sync", lambda e: e.dma_start(out=dtb[:, 0:1], in_=dt_bias[l, :].unsqueeze(1)), writes=["dtb0"])
        k.dma("sync", lambda e: e.dma_start(out=dtb[:, 1:2], in_=a_log[l, :].unsqueeze(1)), writes=["dtb1"])
        k.op("scalar", lambda e: e.activation(out=dtb[:, 1:2], in_=dtb[:, 1:2], func=AF.Exp), reads=["dtb1"], writes=["dtb1"])
        k.op("vector", lambda e: e.tensor_scalar_mul(out=dtb[:, 1:2], in0=dtb[:, 1:2], scalar1=-1.0), reads=["dtb1"], writes=["dtb1"])
        for tc in range(8):
            pj = n % 2
            n += 1

            def fd(e, tc=tc, pj=pj):
                for kb in range(KB):
                    ins = mm(e, pp[pj][0:32, :], wdt[:, kb, :], hT[:, kb, tc * 512:(tc + 1) * 512], kb == 0, kb == KB - 1)
                return ins
            k.op("tensor", fd, reads=["wdt"] + HKEYS[tc * 4:tc * 4 + 4], writes=[("spp", pj)])
            k.op("scalar", lambda e, tc=tc, pj=pj: e.activation(out=dtT[:, tc * 512:(tc + 1) * 512], in_=pp[pj][0:32, :], func=AF.Exp, bias=dtb[:, 0:1]),
                 reads=[("spp", pj), "dtb0"], writes=["dtT"])
        k.op("scalar", lambda e: e.activation(out=dtT[:], in_=dtT[:], func=AF.Ln, bias=1.0), reads=["dtT"], writes=["dtT"])
        k.op("vector", lambda e: e.tensor_scalar_mul(out=Gc[:], in0=dtT[:], scalar1=dtb[:, 1:2]), reads=["dtT", "dtb1"], writes=["Gc"])
        k.op("vector", lambda e: e.tensor_tensor_scan(out=Gc[:], data0=P["ones"][0:32, 0:1].to_broadcast([32, S]), data1=Gc[:], initial=0.0, op0=ALU.mult, op1=ALU.add),
             reads=["Gc", "ones"], writes=["Gc"])
        k.op("vector", lambda e: e.memset(Gprev[:, 0:1], 0.0), writes=["Gprev0"])
        k.op("vector", lambda e: e.tensor_copy(out=Gprev[:, 1:64], in_=Gc[:].rearrange("p (c l) -> p c l", l=64)[:, 0:63, 63]), reads=["Gc"], writes=["Gprev"])
        acs = Gc
        k.op("vector", lambda e: e.tensor_tensor(out=acs[:].rearrange("p (c l) -> p c l", l=64), in0=Gc[:].rearrange("p (c l) -> p c l", l=64),
                                                 in1=Gprev[:].unsqueeze(2).to_broadcast([32, 64, 64]), op=ALU.subtract),
             reads=["Gc", "Gprev", "Gprev0"], writes=["acs", "Gc"])
        k.dma("sync", lambda e: e.dma_start(out=acs_d, in_=acs[:]), reads=["acs"], writes=["acs_d"])
        for src, skey, dst, dkey in ((acs, "acs", P["acol"], "acol"), (dtT, "dtT", P["dtcol"], "dtcol")):
            for q4 in range(4):
                def ft(e, src=src, q4=q4):
                    for i in range(16):
                        c = q4 * 16 + i
                        ins = e.transpose(out=pc[:, i, :], in_=src[0:32, c * 64:(c + 1) * 64], identity=P["ident"][0:32, 0:32])
                    return ins
                k.op("tensor", ft, reads=[skey, "ident"], writes=["spc"])
                k.op("vector", lambda e, dst=dst, q4=q4: e.tensor_copy(out=dst[:, q4 * 16:(q4 + 1) * 16, :], in_=pc[:]), reads=["spc"], writes=[dkey])


def stage_ssd(cx, P, l):
    k = cx.k
    xs_d = cx.dram("xs_d", [S, 2048], BF16)
    Btok_d = cx.dram("Btok_d", [S, 512], BF16)
    BT_d = cx.dram("BT_d", [512, S], BF16)
    CT_d = cx.dram("CT_d", [512, S], BF16)
    acs_d = cx.dram("acs_d", [32, S], F32)
    zs_d = cx.dram("zs_d", [S, 2048], BF16)
    d_skip = cx.dram("d_skip", PARAM_SHAPES["d_skip"])
    ynT_d = cx.dram("ynT_d", [2048, S], BF16)
    c_tri = cx.const("c_tri", (np.arange(64)[:, None] <= np.arange(64)[None, :]).astype(np.float32))
    acol, dtcol = P["acol"], P["dtcol"]
    with k.stage() as es:
        tri = k.sb("tri", [64, 64], F32, es)
        Dbc = k.sb("Dbc", [64, 32], F32, es)
        DI = k.sb("DI", [64, 32, 64], BF16, es)
        rowb = [k.sb("rowb%d" % i, [128, 32, 64], F32, es) for i in range(2)]
        xin = [k.sb("xin%d" % i, [64, 2048], BF16, es) for i in range(2)]
        btok = [k.sb("btok%d" % i, [64, 512], BF16, es) for i in range(2)]
        bct = [k.sb("bct%d" % i, [128, 8, 64], BF16, es) for i in range(2)]
        zs = [k.sb("zsc%d" % i, [64, 2048], BF16, es) for i in range(2)]
        seg_ = [k.sb("seg", [64, 32, 64], F32, es) for _i in range(2)]
        tmp_ = [k.sb("stmp", [64, 32, 64], F32, es) for _i in range(2)]
        sc_ = [k.sb("scoresT", [64, 32, 64], BF16, es) for _i in range(2)]
        eab_ = [k.sb("eab", [128, 32, 64], F32, es) for _i in range(2)]
        CTe_ = [k.sb("CTe", [128, 32, 64], BF16, es) for _i in range(2)]
        CBm_ = [k.sb("CBm", [64, 4, 64], F32, es) for _i in range(2)]
        dd_ = [k.sb("dd", [64, 32], F32, es) for _i in range(2)]
        cdb_ = [k.sb("cdb", [128, 32], F32, es) for _i in range(2)]
        xsc_ = [k.sb("xsc", [64, 32, 64], BF16, es) for _i in range(2)]
        H = k.sb("H", [128, 32, 64], F32, es)
        Hb = k.sb("Hb", [128, 2048], BF16, es)
        yz_ = [k.sb("yz", [64, 2048], F32, es) for _i in range(2)]
        junk_ = [k.sb("junk", [64, 512], F32, es) for _i in range(2)]
        ss_ = [k.sb("ss", [64, 8], F32, es) for _i in range(2)]
        yn_ = [k.sb("yn", [64, 2048], BF16, es) for _i in range(2)]
        ynst = [k.sb("ynst%d" % i, [128, 16, 256], BF16, es) for i in range(2)]
        pcb_ = [k.ps("pcb%d" % i, [64, 4, 64], F32, es) for i in range(2)]
        py = [k.ps("py%d" % i, [64, 512], F32, es) for i in range(2)]
        pst = [k.ps("pst%d" % i, [128, 512], F32, es) for i in range(2)]
        ptr_ = [k.ps("yptr%d" % i, [128, 16, 64], BF16, es) for i in range(2)]
        seg = tmp = sc = eab = CTe = CBm = dd = cdb = xsc = yz = junk = ss = yn = pcb = None
        k.dma("sync", lambda e, seg=seg, tmp=tmp, sc=sc, eab=eab, CTe=CTe, CBm=CBm, dd=dd, cdb=cdb, xsc=xsc, yz=yz, junk=junk, ss=ss, yn=yn, pcb=pcb: e.dma_start(out=tri[:], in_=c_tri), writes=["tri"])
        k.dma("sync", lambda e, seg=seg, tmp=tmp, sc=sc, eab=eab, CTe=CTe, CBm=CBm, dd=dd, cdb=cdb, xsc=xsc, yz=yz, junk=junk, ss=ss, yn=yn, pcb=pcb: e.dma_start(out=Dbc[:], in_=d_skip[l, :].partition_broadcast(64)), writes=["Dbc"])
        k.op("vector", lambda e, seg=seg, tmp=tmp, sc=sc, eab=eab, CTe=CTe, CBm=CBm, dd=dd, cdb=cdb, xsc=xsc, yz=yz, junk=junk, ss=ss, yn=yn, pcb=pcb: e.tensor_tensor(out=DI[:], in0=P["ident"][0:64, 0:64].unsqueeze(1).to_broadcast([64, 32, 64]),
                                                 in1=Dbc[:].unsqueeze(2).to_broadcast([64, 32, 64]), op=ALU.mult), reads=["ident", "Dbc"], writes=["DI"])
        k.op("vector", lambda e, seg=seg, tmp=tmp, sc=sc, eab=eab, CTe=CTe, CBm=CBm, dd=dd, cdb=cdb, xsc=xsc, yz=yz, junk=junk, ss=ss, yn=yn, pcb=pcb: e.memset(H[:], 0.0), writes=[("H", g) for g in range(4)])
        k.op("vector", lambda e, seg=seg, tmp=tmp, sc=sc, eab=eab, CTe=CTe, CBm=CBm, dd=dd, cdb=cdb, xsc=xsc, yz=yz, junk=junk, ss=ss, yn=yn, pcb=pcb: e.memset(Hb[:], 0.0), writes=[("Hb", g) for g in range(4)])
        def front(c):
                b = c % 2
                t0 = c * 64
                seg, tmp, sc, eab, CTe, CBm, dd, cdb, xsc, yz, junk, ss, yn, pcb = (seg_[b], tmp_[b], sc_[b], eab_[b], CTe_[b], CBm_[b], dd_[b], cdb_[b], xsc_[b], yz_[b], junk_[b], ss_[b], yn_[b], pcb_[b])
                ptr = ptr_[b]
                k.dma("sync", lambda e, seg=seg, tmp=tmp, sc=sc, eab=eab, CTe=CTe, CBm=CBm, dd=dd, cdb=cdb, xsc=xsc, yz=yz, junk=junk, ss=ss, yn=yn, pcb=pcb, b=b, t0=t0: e.dma_start(out=rowb[b][:], in_=acs_d[:, t0:t0 + 64].partition_broadcast(128)), writes=[("rowb", b)])
                k.dma("sync", lambda e, seg=seg, tmp=tmp, sc=sc, eab=eab, CTe=CTe, CBm=CBm, dd=dd, cdb=cdb, xsc=xsc, yz=yz, junk=junk, ss=ss, yn=yn, pcb=pcb, b=b, t0=t0: e.dma_start(out=xin[b][:], in_=xs_d[t0:t0 + 64, :]), writes=[("xin", b)])
                k.dma("sync", lambda e, seg=seg, tmp=tmp, sc=sc, eab=eab, CTe=CTe, CBm=CBm, dd=dd, cdb=cdb, xsc=xsc, yz=yz, junk=junk, ss=ss, yn=yn, pcb=pcb, b=b, t0=t0: e.dma_start(out=btok[b][:], in_=Btok_d[t0:t0 + 64, :]), writes=[("btok", b)])
                k.dma("sync", lambda e, seg=seg, tmp=tmp, sc=sc, eab=eab, CTe=CTe, CBm=CBm, dd=dd, cdb=cdb, xsc=xsc, yz=yz, junk=junk, ss=ss, yn=yn, pcb=pcb, b=b, t0=t0: e.dma_start(out=bct[b][:, 0:4, :], in_=BT_d[:, t0:t0 + 64].rearrange("(g n) t -> n g t", n=128)), writes=[("bctB", b)])
                k.dma("sync", lambda e, seg=seg, tmp=tmp, sc=sc, eab=eab, CTe=CTe, CBm=CBm, dd=dd, cdb=cdb, xsc=xsc, yz=yz, junk=junk, ss=ss, yn=yn, pcb=pcb, b=b, t0=t0: e.dma_start(out=bct[b][:, 4:8, :], in_=CT_d[:, t0:t0 + 64].rearrange("(g n) t -> n g t", n=128)), writes=[("bctC", b)])
                k.dma("sync", lambda e, seg=seg, tmp=tmp, sc=sc, eab=eab, CTe=CTe, CBm=CBm, dd=dd, cdb=cdb, xsc=xsc, yz=yz, junk=junk, ss=ss, yn=yn, pcb=pcb, b=b, t0=t0: e.dma_start(out=zs[b][:], in_=zs_d[t0:t0 + 64, :]), writes=[("zsc", b)])
                k.op("vector", lambda e, seg=seg, tmp=tmp, sc=sc, eab=eab, CTe=CTe, CBm=CBm, dd=dd, cdb=cdb, xsc=xsc, yz=yz, junk=junk, ss=ss, yn=yn, pcb=pcb, b=b, c=c: e.tensor_tensor(out=seg[:], in0=rowb[b][0:64, :, :], in1=acol[:, c, :].unsqueeze(2).to_broadcast([64, 32, 64]), op=ALU.subtract),
                     reads=[("rowb", b), "acol"], writes=[("seg", b)])
                k.op("scalar", lambda e, seg=seg, tmp=tmp, sc=sc, eab=eab, CTe=CTe, CBm=CBm, dd=dd, cdb=cdb, xsc=xsc, yz=yz, junk=junk, ss=ss, yn=yn, pcb=pcb: e.activation(out=seg[:], in_=seg[:], func=AF.Exp), reads=[("seg", b)], writes=[("seg", b)])

                def fcb(e, b=b, pcb=pcb):
                    for g in range(4):
                        ins = mm(e, pcb[:, g, :], bct[b][:, g, :], bct[b][:, 4 + g, :], True, True)
                    return ins
                k.op("tensor", fcb, reads=[("bctB", b), ("bctC", b)], writes=[("pcb", b)])
                k.op("vector", lambda e, seg=seg, tmp=tmp, sc=sc, eab=eab, CTe=CTe, CBm=CBm, dd=dd, cdb=cdb, xsc=xsc, yz=yz, junk=junk, ss=ss, yn=yn, pcb=pcb: e.tensor_tensor(out=CBm[:], in0=pcb[:], in1=tri[:].unsqueeze(1).to_broadcast([64, 4, 64]), op=ALU.mult), reads=[("pcb", b), "tri"], writes=[("CBm", b)])
                for g in range(4):
                    k.op("vector", lambda e, seg=seg, tmp=tmp, sc=sc, eab=eab, CTe=CTe, CBm=CBm, dd=dd, cdb=cdb, xsc=xsc, yz=yz, junk=junk, ss=ss, yn=yn, pcb=pcb, g=g: e.scalar_tensor_tensor(out=tmp[:, g * 8:(g + 1) * 8, :], in0=seg[:, g * 8:(g + 1) * 8, :], scalar=1.0,
                                                                         in1=CBm[:, g, :].unsqueeze(1).to_broadcast([64, 8, 64]), op0=ALU.min, op1=ALU.mult),
                         reads=[("seg", b), ("CBm", b)], writes=[("stmp", b, g)])
                k.op("vector", lambda e, seg=seg, tmp=tmp, sc=sc, eab=eab, CTe=CTe, CBm=CBm, dd=dd, cdb=cdb, xsc=xsc, yz=yz, junk=junk, ss=ss, yn=yn, pcb=pcb, c=c: e.tensor_tensor(out=sc[:], in0=tmp[:], in1=dtcol[:, c, :].unsqueeze(2).to_broadcast([64, 32, 64]), op=ALU.mult),
                     reads=[("stmp", b, g) for g in range(4)] + ["dtcol"], writes=[("scoresT", b)])
                k.op("scalar", lambda e, seg=seg, tmp=tmp, sc=sc, eab=eab, CTe=CTe, CBm=CBm, dd=dd, cdb=cdb, xsc=xsc, yz=yz, junk=junk, ss=ss, yn=yn, pcb=pcb, b=b: e.activation(out=eab[:], in_=rowb[b][:], func=AF.Exp), reads=[("rowb", b)], writes=[("eab", b)])
                for g in range(4):
                    k.op("gpsimd", lambda e, seg=seg, tmp=tmp, sc=sc, eab=eab, CTe=CTe, CBm=CBm, dd=dd, cdb=cdb, xsc=xsc, yz=yz, junk=junk, ss=ss, yn=yn, pcb=pcb, g=g, b=b: e.tensor_tensor(out=CTe[:, g * 8:(g + 1) * 8, :], in0=eab[:, g * 8:(g + 1) * 8, :],
                                                                        in1=bct[b][:, 4 + g, :].unsqueeze(1).to_broadcast([128, 8, 64]), op=ALU.mult),
                         reads=[("eab", b), ("bctC", b)], writes=[("CTe", b, g)])
                k.op("vector", lambda e, seg=seg, tmp=tmp, sc=sc, eab=eab, CTe=CTe, CBm=CBm, dd=dd, cdb=cdb, xsc=xsc, yz=yz, junk=junk, ss=ss, yn=yn, pcb=pcb, b=b, c=c: e.tensor_tensor(out=dd[:], in0=rowb[b][0:64, :, 63], in1=acol[:, c, :], op=ALU.subtract), reads=[("rowb", b), "acol"], writes=[("dd", b)])
                k.op("scalar", lambda e, seg=seg, tmp=tmp, sc=sc, eab=eab, CTe=CTe, CBm=CBm, dd=dd, cdb=cdb, xsc=xsc, yz=yz, junk=junk, ss=ss, yn=yn, pcb=pcb: e.activation(out=dd[:], in_=dd[:], func=AF.Exp), reads=[("dd", b)], writes=[("dd", b)])
                k.op("vector", lambda e, seg=seg, tmp=tmp, sc=sc, eab=eab, CTe=CTe, CBm=CBm, dd=dd, cdb=cdb, xsc=xsc, yz=yz, junk=junk, ss=ss, yn=yn, pcb=pcb, c=c: e.tensor_tensor(out=dd[:], in0=dd[:], in1=dtcol[:, c, :], op=ALU.mult), reads=[("dd", b), "dtcol"], writes=[("dd", b)])
                k.op("gpsimd", lambda e, seg=seg, tmp=tmp, sc=sc, eab=eab, CTe=CTe, CBm=CBm, dd=dd, cdb=cdb, xsc=xsc, yz=yz, junk=junk, ss=ss, yn=yn, pcb=pcb, b=b: e.tensor_tensor(out=xsc[:], in0=xin[b][:].rearrange("p (h n) -> p h n", n=64), in1=dd[:].unsqueeze(2).to_broadcast([64, 32, 64]), op=ALU.mult),
                     reads=[("xin", b), ("dd", b)], writes=[("xsc", b)])
                k.op("scalar", lambda e, seg=seg, tmp=tmp, sc=sc, eab=eab, CTe=CTe, CBm=CBm, dd=dd, cdb=cdb, xsc=xsc, yz=yz, junk=junk, ss=ss, yn=yn, pcb=pcb, b=b: e.activation(out=cdb[:], in_=rowb[b][:, :, 63], func=AF.Exp), reads=[("rowb", b)], writes=[("cdb", b)])

        def back(c):
                b = c % 2
                t0 = c * 64
                seg, tmp, sc, eab, CTe, CBm, dd, cdb, xsc, yz, junk, ss, yn, pcb = (seg_[b], tmp_[b], sc_[b], eab_[b], CTe_[b], CBm_[b], dd_[b], cdb_[b], xsc_[b], yz_[b], junk_[b], ss_[b], yn_[b], pcb_[b])
                ptr = ptr_[b]
                for g in range(4):
                    pj = g % 2

                    def fy(e, g=g, b=b, pj=pj, sc=sc, CTe=CTe):
                        for r in range(8):
                            h = g * 8 + r
                            o = py[pj][:, r * 64:(r + 1) * 64]
                            xh = xin[b][:, h * 64:(h + 1) * 64]
                            mm(e, o, sc[:, h, :], xh, True, False)
                            mm(e, o, CTe[:, h, :], Hb[:, h * 64:(h + 1) * 64], False, False)
                            ins = mm(e, o, DI[:, h, :], xh, False, True)
                        return ins
                    k.op("tensor", fy, reads=[("scoresT", b), ("xin", b), ("CTe", b, g), ("Hb", g), "DI"], writes=[("py", pj)])
                    k.op("tensor", lambda e, seg=seg, tmp=tmp, sc=sc, eab=eab, CTe=CTe, CBm=CBm, dd=dd, cdb=cdb, xsc=xsc, yz=yz, junk=junk, ss=ss, yn=yn, pcb=pcb, g=g, b=b, pj=pj: mm(e, pst[pj][:], btok[b][:, g * 128:(g + 1) * 128], xsc[:, g * 8:(g + 1) * 8, :], True, True),
                         reads=[("btok", b), ("xsc", b)], writes=[("pst", pj)])
                    k.op("vector", lambda e, seg=seg, tmp=tmp, sc=sc, eab=eab, CTe=CTe, CBm=CBm, dd=dd, cdb=cdb, xsc=xsc, yz=yz, junk=junk, ss=ss, yn=yn, pcb=pcb, g=g: e.tensor_tensor(out=H[:, g * 8:(g + 1) * 8, :], in0=H[:, g * 8:(g + 1) * 8, :],
                                                                  in1=cdb[:, g * 8:(g + 1) * 8].unsqueeze(2).to_broadcast([128, 8, 64]), op=ALU.mult),
                         reads=[("H", g), ("cdb", b)], writes=[("H", g)])
                    k.op("vector", lambda e, seg=seg, tmp=tmp, sc=sc, eab=eab, CTe=CTe, CBm=CBm, dd=dd, cdb=cdb, xsc=xsc, yz=yz, junk=junk, ss=ss, yn=yn, pcb=pcb, g=g, pj=pj: e.tensor_tensor(out=H[:, g * 8:(g + 1) * 8, :], in0=H[:, g * 8:(g + 1) * 8, :],
                                                                         in1=pst[pj][:].rearrange("p (h n) -> p h n", n=64), op=ALU.add),
                         reads=[("H", g), ("pst", pj)], writes=[("H", g)])
                    k.op("scalar", lambda e, seg=seg, tmp=tmp, sc=sc, eab=eab, CTe=CTe, CBm=CBm, dd=dd, cdb=cdb, xsc=xsc, yz=yz, junk=junk, ss=ss, yn=yn, pcb=pcb, g=g: e.copy(out=Hb[:, g * 512:(g + 1) * 512], in_=H[:, g * 8:(g + 1) * 8, :]), reads=[("H", g)], writes=[("Hb", g)])
                    k.op("vector", lambda e, seg=seg, tmp=tmp, sc=sc, eab=eab, CTe=CTe, CBm=CBm, dd=dd, cdb=cdb, xsc=xsc, yz=yz, junk=junk, ss=ss, yn=yn, pcb=pcb, g=g, b=b, pj=pj: e.tensor_tensor(out=yz[:, g * 512:(g + 1) * 512], in0=py[pj][:], in1=zs[b][:, g * 512:(g + 1) * 512], op=ALU.mult),
                         reads=[("py", pj), ("zsc", b)], writes=[("yz", b, g)])
                    k.op("scalar", lambda e, seg=seg, tmp=tmp, sc=sc, eab=eab, CTe=CTe, CBm=CBm, dd=dd, cdb=cdb, xsc=xsc, yz=yz, junk=junk, ss=ss, yn=yn, pcb=pcb, g=g: e.activation(out=junk[:], in_=yz[:, g * 512:(g + 1) * 512], func=AF.Square, accum_out=ss[:, g:g + 1]),
                         reads=[("yz", b, g)], writes=[("junk", b), ("ss", b, g)])
                k.op("scalar", lambda e, ss=ss: e.activation(out=ss[:, 4:8], in_=ss[:, 0:4], func=AF.Ln, scale=1.0 / 512, bias=RMS_EPS), reads=[("ss", b, g) for g in range(4)], writes=[("rstd", b)])
                k.op("scalar", lambda e, ss=ss: e.activation(out=ss[:, 4:8], in_=ss[:, 4:8], func=AF.Exp, scale=-0.5), reads=[("rstd", b)], writes=[("rstd", b)])
                for g in range(4):
                    k.op("scalar", lambda e, seg=seg, tmp=tmp, sc=sc, eab=eab, CTe=CTe, CBm=CBm, dd=dd, cdb=cdb, xsc=xsc, yz=yz, junk=junk, ss=ss, yn=yn, pcb=pcb, g=g: e.activation(out=yn[:, g * 512:(g + 1) * 512], in_=yz[:, g * 512:(g + 1) * 512], func=AF.Copy, scale=ss[:, 4 + g:5 + g]),
                         reads=[("yz", b, g), ("rstd", b)], writes=[("yn", b, g)])

                def ftr(e, yn=yn, ptr=ptr):
                    for kb in range(16):
                        ins = e.transpose(out=ptr[:, kb, :], in_=yn[0:64, kb * 128:(kb + 1) * 128], identity=P["identb"][0:64, 0:64])
                    return ins
                k.op("tensor", ftr, reads=[("yn", b, g) for g in range(4)] + ["identb"], writes=[("yptr", b)])
                sbi = (c // 4) % 2
                k.op("scalar", lambda e, seg=seg, tmp=tmp, sc=sc, eab=eab, CTe=CTe, CBm=CBm, dd=dd, cdb=cdb, xsc=xsc, yz=yz, junk=junk, ss=ss, yn=yn, pcb=pcb, c=c, sbi=sbi: e.copy(out=ynst[sbi][:, :, (c % 4) * 64:(c % 4 + 1) * 64], in_=ptr[:]), reads=[("yptr", b)], writes=[("ynst", sbi)])
                if c % 4 == 3:
                    tcn = c // 4
                    k.dma("scalar", lambda e, seg=seg, tmp=tmp, sc=sc, eab=eab, CTe=CTe, CBm=CBm, dd=dd, cdb=cdb, xsc=xsc, yz=yz, junk=junk, ss=ss, yn=yn, pcb=pcb, tcn=tcn, sbi=sbi: e.dma_start(out=ynT_d[:, tcn * 256:(tcn + 1) * 256].rearrange("(kb p) t -> p kb t", p=128), in_=ynst[sbi][:]),
                          reads=[("ynst", sbi)], writes=[("ynT_d", tcn)])

        front(0)
        for c in range(64):
            if c + 1 < 64:
                front(c + 1)
            back(c)


class ResLN:
    def __init__(self, cx, P, es, gbc_key, pool_ok=False):
        k = cx.k
        self.cx, self.P, self.gk = cx, P, gbc_key
        self.eng = "gpsimd" if pool_ok else "vector"
        self.xt = [k.sb("rx%d" % i, [128, D], F32, es) for i in range(2)]
        self.r = [k.sb("rr%d" % i, [128, D], F32, es) for i in range(2)]
        self.st = [k.sb("rst%d" % i, [128, 2, 6], F32, es) for i in range(2)]
        self.mv = [k.sb("rmv%d" % i, [128, 4], F32, es) for i in range(2)]
        self.n = 0

    def load_params(self, g_ap, b_ap):
        k, P = self.cx.k, self.P
        k.dma("sync", lambda e: e.dma_start(out=P["lng"][:], in_=g_ap.partition_broadcast(128)), writes=["lng"])
        k.dma("sync", lambda e: e.dma_start(out=P["lnb"][:], in_=b_ap.partition_broadcast(128)), writes=["lnb"])

    def emit(self, tb, mix_halves, mix_keys, x_src, x_dst):
        k, P = self.cx.k, self.P
        b = self.n % 2
        self.n += 1
        xt, r, st, mv = self.xt[b], self.r[b], self.st[b], self.mv[b]
        gbc = P[self.gk]
        k.dma("sync", lambda e: e.dma_start(out=xt[:], in_=x_src[tb * 128:(tb + 1) * 128, :]), writes=[("rx", b)])
        eng = self.eng
        for hf in range(2):
            k.op(eng, lambda e, hf=hf: e.tensor_tensor(out=r[:, hf * 512:(hf + 1) * 512], in0=mix_halves[hf], in1=gbc[:, hf * 512:(hf + 1) * 512], op=ALU.mult),
                 reads=[mix_keys[hf], self.gk], writes=[("rr", b, hf)])
        if eng == "vector":
            k.op("vector", lambda e: e.scalar_tensor_tensor(out=r[:], in0=xt[:], scalar=ALPHA, in1=r[:], op0=ALU.mult, op1=ALU.add),
                 reads=[("rx", b), ("rr", b, 0), ("rr", b, 1)], writes=[("rr", b)])
        else:
            k.op(eng, lambda e: e.tensor_scalar_mul(out=xt[:], in0=xt[:], scalar1=ALPHA), reads=[("rx", b)], writes=[("rx", b)])
            k.op(eng, lambda e: e.tensor_tensor(out=r[:], in0=r[:], in1=xt[:], op=ALU.add), reads=[("rx", b), ("rr", b, 0), ("rr", b, 1)], writes=[("rr", b)])
        k.op("vector", lambda e: e.bn_stats(out=st[:, 0, :], in_=r[:, 0:512]), reads=[("rr", b)], writes=[("rst", b, 0)])
        k.op("vector", lambda e: e.bn_stats(out=st[:, 1, :], in_=r[:, 512:1024]), reads=[("rr", b)], writes=[("rst", b, 1)])
        k.op("vector", lambda e: e.bn_aggr(out=mv[:, 0:2], in_=st[:]), reads=[("rst", b, 0), ("rst", b, 1)], writes=[("rmv", b)])
        k.op("scalar", lambda e: e.activation(out=mv[:, 2:3], in_=mv[:, 1:2], func=AF.Ln, bias=LN_EPS), reads=[("rmv", b)], writes=[("rmv", b)])
        k.op("scalar", lambda e: e.activation(out=mv[:, 2:3], in_=mv[:, 2:3], func=AF.Exp, scale=-0.5), reads=[("rmv", b)], writes=[("rmv", b)])
        k.op(eng, lambda e: e.tensor_scalar(out=xt[:], in0=r[:], scalar1=mv[:, 0:1], scalar2=mv[:, 2:3], op0=ALU.subtract, op1=ALU.mult),
             reads=[("rr", b), ("rmv", b), ("rx", b)], writes=[("rx", b)])
        k.op("gpsimd", lambda e: e.tensor_tensor(out=xt[:], in0=xt[:], in1=P["lng"][:], op=ALU.mult), reads=[("rx", b), "lng"], writes=[("rx", b)])
        k.op("gpsimd", lambda e: e.tensor_tensor(out=xt[:], in0=xt[:], in1=P["lnb"][:], op=ALU.add), reads=[("rx", b), "lnb"], writes=[("rx", b)])
        k.dma("gpsimd", lambda e: e.dma_start(out=x_dst[tb * 128:(tb + 1) * 128, :], in_=xt[:]), reads=[("rx", b)], writes=[("x_dst", tb)])


def stage_merge(cx, P, l, x_src, x_dst):
    k = cx.k
    attT_d = cx.dram("attT_d", [1024, S], BF16)
    ynT_d = cx.dram("ynT_d", [2048, S], BF16)
    ypT_d = cx.dram("ypT_d", [1024, S], BF16)
    gT_d = cx.dram("gT_d", [3072, S], BF16)
    w_attn_o = cx.dram("w_attn_o", PARAM_SHAPES["w_attn_o"])
    w_ssm_o = cx.dram("w_ssm_o", PARAM_SHAPES["w_ssm_o"])
    w_out = cx.dram("w_out", PARAM_SHAPES["w_out"])
    ssm_norm_w = cx.dram("ssm_norm_w", PARAM_SHAPES["ssm_norm_w"])
    ln_g = cx.dram("ln_mix_g", PARAM_SHAPES["ln_mix_g"])
    ln_b = cx.dram("ln_mix_b", PARAM_SHAPES["ln_mix_b"])
    TC = 256
    with k.stage() as es:
        wao = k.sb("wao", [128, 8, D], BF16, es)
        wso = k.sb("wso", [128, 16, D], BF16, es)
        wout = k.sb("wout", [128, 8, D], BF16, es)
        nw = k.sb("nw", [128, 16], F32, es)
        wstg = [k.sb("wstg%d" % i, [128, D], F32, es) for i in range(4)]
        at = [k.sb("m_at%d" % i, [128, 8, TC], BF16, es) for i in range(2)]
        yT = [k.sb("m_yT%d" % i, [128, 16, TC], BF16, es) for i in range(2)]
        yp = [k.sb("m_yp%d" % i, [128, 8, TC], BF16, es) for i in range(2)]
        gt = [k.sb("m_gt%d" % i, [128, 24, TC], BF16, es) for i in range(2)]
        m1 = [k.sb("m_m1%d" % i, [128, TC], F32, es) for i in range(2)]
        m2 = [k.sb("m_m2%d" % i, [128, TC], F32, es) for i in range(2)]
        m3 = [k.sb("m_m3%d" % i, [128, TC], F32, es) for i in range(2)]
        mg = [k.sb("m_mg%d" % i, [128, 8, TC], BF16, es) for i in range(2)]
        p1 = [k.ps("m_p1%d" % i, [128, TC], F32, es) for i in range(2)]
        p2 = [k.ps("m_p2%d" % i, [128, TC], F32, es) for i in range(2)]
        pm = [k.ps("m_pm%d" % i, [128, 512], F32, es) for i in range(4)]
        rl = ResLN(cx, P, es, "gbc_m")
        rl.load_params(ln_g[l, :], ln_b[l, :])
        for kb in range(8):
            k.dma("gpsimd", lambda e, kb=kb: e.dma_start(out=wao[:, kb, :], in_=w_attn_o[l, kb * 128:(kb + 1) * 128, :]), writes=[("wao", kb)])
        for kb in range(8):
            k.dma("gpsimd", lambda e, kb=kb: e.dma_start(out=wout[:, kb, :], in_=w_out[l, kb * 128:(kb + 1) * 128, :]), writes=[("wout", kb)])
        k.dma("sync", lambda e: e.dma_start(out=nw[:], in_=ssm_norm_w[l, :].rearrange("(kb p) -> p kb", p=128)), writes=["nw"])
        for kb in range(16):
            b = kb % 4
            k.dma("sync", lambda e, kb=kb, b=b: e.dma_start(out=wstg[b][:], in_=w_ssm_o[l, kb * 128:(kb + 1) * 128, :]), writes=[("wstg", b)])
            k.op("vector", lambda e, kb=kb, b=b: e.tensor_scalar_mul(out=wso[:, kb, :], in0=wstg[b][:], scalar1=nw[:, kb:kb + 1]), reads=[("wstg", b), "nw"], writes=[("wso", kb)])
        wso_keys = [("wso", kb) for kb in range(16)]
        npm = 0
        for tc in range(S // TC):
            b = tc % 2
            c0 = tc * TC
            k.dma("sync", lambda e, b=b, c0=c0: e.dma_start(out=at[b][:], in_=attT_d[:, c0:c0 + TC].rearrange("(kb p) t -> p kb t", p=128)), writes=[("m_at", b)])
            k.dma("sync", lambda e, b=b, c0=c0: e.dma_start(out=yT[b][:], in_=ynT_d[:, c0:c0 + TC].rearrange("(kb p) t -> p kb t", p=128)), writes=[("m_yT", b)])
            k.dma("sync", lambda e, b=b, c0=c0: e.dma_start(out=yp[b][:], in_=ypT_d[:, c0:c0 + TC].rearrange("(kb p) t -> p kb t", p=128)), writes=[("m_yp", b)])
            k.dma("sync", lambda e, b=b, c0=c0: e.dma_start(out=gt[b][:], in_=gT_d[:, c0:c0 + TC].rearrange("(kb p) t -> p kb t", p=128)), writes=[("m_gt", b)])
            for cb in range(8):
                j = cb % 2

                def f1(e, b=b, cb=cb, j=j):
                    for kb in range(8):
                        ins = mm(e, p1[j][:], wao[:, kb, cb * 128:(cb + 1) * 128], at[b][:, kb, :], kb == 0, kb == 7)
                    return ins
                k.op("tensor", f1, reads=[("wao", kb_) for kb_ in range(8)] + [("m_at", b)], writes=[("m_p1", j)])

                def f2(e, b=b, cb=cb, j=j):
                    for kb in range(16):
                        ins = mm(e, p2[j][:], wso[:, kb, cb * 128:(cb + 1) * 128], yT[b][:, kb, :], kb == 0, kb == 15)
                    return ins
                k.op("tensor", f2, reads=wso_keys + [("m_yT", b)], writes=[("m_p2", j)])
                k.op("vector", lambda e, b=b, cb=cb, j=j: e.tensor_tensor(out=m1[j][:], in0=p1[j][:], in1=gt[b][:, cb, :], op=ALU.mult), reads=[("m_p1", j), ("m_gt", b)], writes=[("m_m1", j)])
                k.op("vector", lambda e, b=b, cb=cb, j=j: e.tensor_tensor(out=m2[j][:], in0=p2[j][:], in1=gt[b][:, 16 + cb, :], op=ALU.mult), reads=[("m_p2", j), ("m_gt", b)], writes=[("m_m2", j)])
                k.op("gpsimd", lambda e, b=b, cb=cb, j=j: e.tensor_tensor(out=m3[j][:], in0=yp[b][:, cb, :], in1=gt[b][:, 8 + cb, :], op=ALU.mult), reads=[("m_yp", b), ("m_gt", b)], writes=[("m_m3", j)])
                k.op("vector", lambda e, j=j: e.tensor_tensor(out=m1[j][:], in0=m1[j][:], in1=m2[j][:], op=ALU.add), reads=[("m_m1", j), ("m_m2", j)], writes=[("m_m1", j)])
                k.op("gpsimd", lambda e, b=b, cb=cb, j=j: e.tensor_tensor(out=mg[b][:, cb, :], in0=m1[j][:], in1=m3[j][:], op=ALU.add), reads=[("m_m1", j), ("m_m3", j)], writes=[("m_mg", b, cb)])
            for t2 in range(TC // 128):
                tb = tc * (TC // 128) + t2
                halves, keys = [], []
                for hf in range(2):
                    pj = npm % 4
                    npm += 1

                    def f3(e, b=b, t2=t2, hf=hf, pj=pj):
                        for kb in range(8):
                            ins = mm(e, pm[pj][:], mg[b][:, kb, t2 * 128:(t2 + 1) * 128], wout[:, kb, hf * 512:(hf + 1) * 512], kb == 0, kb == 7)
                        return ins
                    k.op("tensor", f3, reads=[("wout", kb_) for kb_ in range(8)] + [("m_mg", b, cb) for cb in range(8)], writes=[("m_pm", pj)])
                    halves.append(pm[pj][:])
                    keys.append(("m_pm", pj))
                rl.emit(tb, halves, keys, x_src, x_dst)


def stage_moe(cx, P, l, x_src, x_dst):
    k = cx.k
    w_router = cx.dram("w_router", PARAM_SHAPES["w_router"])
    b_router = cx.dram("b_router", PARAM_SHAPES["b_router"])
    w1 = cx.dram("w_exp_gate", PARAM_SHAPES["w_exp_gate"])
    w3 = cx.dram("w_exp_up", PARAM_SHAPES["w_exp_up"])
    w2 = cx.dram("w_exp_down", PARAM_SHAPES["w_exp_down"])
    ln_g = cx.dram("ln_ffn_g", PARAM_SHAPES["ln_ffn_g"])
    ln_b = cx.dram("ln_ffn_b", PARAM_SHAPES["ln_ffn_b"])
    with contextlib.ExitStack() as eso:
        hT = k.sb("h2T", [128, KB, S], BF16, eso)
        wd = k.sb("wd", [128, NTB, 16], F32, eso)
        with k.stage() as es:
            wr = k.sb("wr", [128, KB, 16], F32, es)
            brb = k.sb("brb", [128, 16], F32, es)
            pl = [k.ps("pl%d" % i, [128, 16], F32, es) for i in range(2)]
            T = {n: [k.sb("rt_%s%d" % (n, i), shp, F32, es) for i in range(2)] for n, shp in
                 (("e", [128, 16]), ("pr", [128, 16]), ("sel", [128, 16]), ("ge", [128, 16]), ("sel2", [128, 16]), ("pw", [128, 16]), ("s", [128, 16]))}
            k.dma("sync", lambda e: e.dma_start(out=wr[:], in_=w_router.rearrange("(kb p) n -> p kb n", p=128)), writes=["wr"])
            k.dma("sync", lambda e: e.dma_start(out=brb[:], in_=b_router.partition_broadcast(128)), writes=["brb"])

            def router(tb, hkeys, h2f):
                b = tb % 2
                e_, pr, sel, ge, sel2, pw, s_ = (T[n][b] for n in ("e", "pr", "sel", "ge", "sel2", "pw", "s"))
                kk = ("rt", b)

                def f(e):
                    for kb in range(KB):
                        ins = mm(e, pl[b][:], h2f[:, kb, :], wr[:, kb, :], kb == 0, kb == KB - 1)
                    return ins
                k.op("tensor", f, reads=hkeys + ["wr"], writes=[("pl", b)])
                return lambda: router_dve(tb, b, e_, pr, sel, ge, sel2, pw, s_, kk)

            def router_dve(tb, b, e_, pr, sel, ge, sel2, pw, s_, kk):
                V = lambda fn, r=(), w=(): k.op("vector", fn, reads=list(r) + [kk], writes=list(w) + [kk])
                V(lambda e: e.tensor_reduce(out=s_[:, 0:1], in_=pl[b][:], axis=mybir.AxisListType.X, op=ALU.max), r=[("pl", b)])
                V(lambda e: e.tensor_scalar_mul(out=s_[:, 1:2], in0=s_[:, 0:1], scalar1=-1.0))
                k.op("scalar", lambda e: e.activation(out=e_[:], in_=pl[b][:], func=AF.Exp, bias=s_[:, 1:2], accum_out=s_[:, 2:3]), reads=[("pl", b), kk], writes=[kk])
                V(lambda e: e.reciprocal(out=s_[:, 3:4], in_=s_[:, 2:3]))
                V(lambda e: e.tensor_scalar_mul(out=pr[:], in0=e_[:], scalar1=s_[:, 3:4]))
                V(lambda e: e.tensor_tensor(out=sel[:], in0=pr[:], in1=brb[:], op=ALU.add), r=["brb"])
                s3 = sel[:].rearrange("p (g j) -> p g j", j=4)
                V(lambda e: e.tensor_reduce(out=s_[:, 4:8], in_=s3, axis=mybir.AxisListType.X, op=ALU.max))
                V(lambda e: e.tensor_tensor(out=ge[:].rearrange("p (g j) -> p g j", j=4), in0=s3, in1=s_[:, 4:8].unsqueeze(2).to_broadcast([128, 4, 4]), op=ALU.is_ge))
                V(lambda e: e.scalar_tensor_tensor(out=sel2[:], in0=ge[:], scalar=-1e9, in1=sel[:], op0=ALU.mult, op1=ALU.add))
                V(lambda e: e.tensor_reduce(out=s_[:, 8:12], in_=sel2[:].rearrange("p (g j) -> p g j", j=4), axis=mybir.AxisListType.X, op=ALU.max))
                V(lambda e: e.tensor_tensor(out=s_[:, 4:8], in0=s_[:, 4:8], in1=s_[:, 8:12], op=ALU.add))
                V(lambda e: e.tensor_reduce(out=s_[:, 12:13], in_=s_[:, 4:8], axis=mybir.AxisListType.X, op=ALU.max))
                V(lambda e: e.tensor_scalar(out=s_[:, 4:8], in0=s_[:, 4:8], scalar1=s_[:, 12:13], scalar2=None, op0=ALU.is_ge))
                V(lambda e: e.tensor_tensor(out=ge[:].rearrange("p (g j) -> p g j", j=4), in0=s3, in1=s_[:, 8:12].unsqueeze(2).to_broadcast([128, 4, 4]), op=ALU.is_ge))
                V(lambda e: e.tensor_tensor(out=ge[:].rearrange("p (g j) -> p g j", j=4), in0=ge[:].rearrange("p (g j) -> p g j", j=4),
                                            in1=s_[:, 4:8].unsqueeze(2).to_broadcast([128, 4, 4]), op=ALU.mult))
                V(lambda e: e.tensor_tensor(out=pw[:], in0=pr[:], in1=ge[:], op=ALU.mult))
                V(lambda e: e.tensor_reduce(out=s_[:, 13:14], in_=pw[:], axis=mybir.AxisListType.X, op=ALU.add))
                V(lambda e: e.reciprocal(out=s_[:, 13:14], in_=s_[:, 13:14]))
                V(lambda e: e.tensor_scalar_mul(out=wd[:, tb, :], in0=pw[:], scalar1=s_[:, 13:14]), w=[("wd", tb)])
            emit_lnt(cx, P, es, x_src, hT, 24, 32, per_block=router)
        TCH = 1024
        NB = TCH // 128
        with k.stage() as es:
            rl = ResLN(cx, P, es, "gbc_f")
            rl.load_params(ln_g[l, :], ln_b[l, :])
            W1 = [k.sb("W1_%d" % i, [128, KB, 512], BF16, es) for i in range(2)]
            W3 = [k.sb("W3_%d" % i, [128, KB, 512], BF16, es) for i in range(2)]
            W2 = [k.sb("W2_%d" % i, [128, 4, D], BF16, es) for i in range(2)]
            acc = k.sb("acc", [128, NB, D], F32, es)
            aT = [k.sb("aT%d" % i, [128, 4, 512], BF16, es) for i in range(2)]
            sg = [k.sb("sg%d" % i, [128, 512], BF16, es) for i in range(2)]
            pg = [k.ps("pg%d" % i, [128, 512], F32, es) for i in range(2)]
            pu = [k.ps("pu%d" % i, [128, 512], F32, es) for i in range(2)]
            po = [k.ps("po%d" % i, [128, 512], F32, es) for i in range(2)]
            n = 0
            na = 0
            no = 0
            ne = 0
            pend_ln = []
            for tp in range(S // TCH):
                for ex in range(16):
                    wb = ne % 2
                    ne += 1
                    k.dma("gpsimd", lambda e, ex=ex, wb=wb: e.dma_start(out=W1[wb][:], in_=w1[l, ex].rearrange("(kb p) n -> p kb n", p=128)), writes=[("W1", wb)])
                    k.dma("gpsimd", lambda e, ex=ex, wb=wb: e.dma_start(out=W3[wb][:], in_=w3[l, ex].rearrange("(kb p) n -> p kb n", p=128)), writes=[("W3", wb)])
                    k.dma("gpsimd", lambda e, ex=ex, wb=wb: e.dma_start(out=W2[wb][:], in_=w2[l, ex].rearrange("(fb p) n -> p fb n", p=128)), writes=[("W2", wb)])
                    for sub in range(TCH // 512):
                        t0 = tp * TCH + sub * 512
                        ab = na % 2
                        na += 1
                        hk = HKEYS[t0 // 128:t0 // 128 + 4]
                        for fb in range(4):
                            j = n % 2
                            n += 1
                            if pend_ln:
                                tb_, tbl_ = pend_ln.pop(0)
                                rl.emit(tb_, [acc[:, tbl_, 0:512], acc[:, tbl_, 512:1024]], [("acc", tbl_, 0), ("acc", tbl_, 1)], x_src, x_dst)

                            def fg(e, wb=wb, fb=fb, j=j, t0=t0):
                                for kb in range(KB):
                                    ins = mm(e, pg[j][:], W1[wb][:, kb, fb * 128:(fb + 1) * 128], hT[:, kb, t0:t0 + 512], kb == 0, kb == KB - 1)
                                return ins
                            k.op("tensor", fg, reads=[("W1", wb)] + hk, writes=[("pg", j)])

                            def fu(e, wb=wb, fb=fb, j=j, t0=t0):
                                for kb in range(KB):
                                    ins = mm(e, pu[j][:], W3[wb][:, kb, fb * 128:(fb + 1) * 128], hT[:, kb, t0:t0 + 512], kb == 0, kb == KB - 1)
                                return ins
                            k.op("tensor", fu, reads=[("W3", wb)] + hk, writes=[("pu", j)])
                            k.op("scalar", lambda e, j=j: e.activation(out=sg[j][:], in_=pg[j][:], func=AF.Silu), reads=[("pg", j)], writes=[("sg", j)])
                            k.op("vector", lambda e, j=j, ab=ab, fb=fb: e.tensor_tensor(out=aT[ab][:, fb, :], in0=sg[j][:], in1=pu[j][:], op=ALU.mult),
                                 reads=[("sg", j), ("pu", j)], writes=[("aT", ab, fb)])
                        for t4 in range(4):
                            tbl = sub * 4 + t4
                            tb = t0 // 128 + t4
                            for hf in range(2):
                                oj = no % 2
                                no += 1

                                def fo(e, wb=wb, ab=ab, t4=t4, hf=hf, oj=oj):
                                    for fb in range(4):
                                        ins = mm(e, po[oj][:], aT[ab][:, fb, t4 * 128:(t4 + 1) * 128], W2[wb][:, fb, hf * 512:(hf + 1) * 512], fb == 0, fb == 3)
                                    return ins
                                k.op("tensor", fo, reads=[("W2", wb)] + [("aT", ab, fb) for fb in range(4)], writes=[("po", oj)])
                                dst = acc[:, tbl, hf * 512:(hf + 1) * 512]
                                if ex == 0:
                                    k.op("vector", lambda e, oj=oj, dst=dst, tb=tb, ex=ex: e.tensor_scalar_mul(out=dst, in0=po[oj][:], scalar1=wd[:, tb, ex:ex + 1]),
                                         reads=[("po", oj), ("wd", tb)], writes=[("acc", tbl, hf)])
                                else:
                                    k.op("vector", lambda e, oj=oj, dst=dst, tb=tb, ex=ex: e.scalar_tensor_tensor(out=dst, in0=po[oj][:], scalar=wd[:, tb, ex:ex + 1], in1=dst,
                                                                                                                  op0=ALU.mult, op1=ALU.add),
                                         reads=[("po", oj), ("wd", tb), ("acc", tbl, hf)], writes=[("acc", tbl, hf)])
                        if ex == 15:
                            for t4 in range(4):
                                tbl = sub * 4 + t4
                                pend_ln.append((tp * NB + tbl, tbl))
            for tb_, tbl_ in pend_ln:
                rl.emit(tb_, [acc[:, tbl_, 0:512], acc[:, tbl_, 512:1024]], [("acc", tbl_, 0), ("acc", tbl_, 1)], x_src, x_dst)


PARAM_NAMES = list(PARAM_SHAPES.keys())


def build_program():
    cx = Ctx(ext_in=["x", "c"] + PARAM_NAMES, ext_out=["out"])
    k = cx.k
    for n in PARAM_NAMES:
        cx.dram(n, PARAM_SHAPES[n])
    x_cur = cx.dram("x", [S, D])
    cx.dram("c", [D])
    P = setup_consts(cx)
    for l in range(DEPTH):
        stage_mod(cx, P, l)
        with contextlib.ExitStack() as eso:
            hT = k.sb("hT", [128, KB, S], BF16, eso)
            with k.stage() as es:
                emit_lnt(cx, P, es, x_cur, hT, 0, 8)
            stage_att(cx, P, hT, l)
            stage_gates(cx, P, hT, l)
            stage_z(cx, P, hT, l)
            stage_pool(cx, P, hT, l)
            stage_ssdprep(cx, P, hT, l)
        stage_ssd(cx, P, l)
        x_mid = cx.dram("x_mid", [S, D])
        stage_merge(cx, P, l, x_cur, x_mid)
        x_next = cx.dram("out" if l == DEPTH - 1 else "x_l%d" % l, [S, D])
        stage_moe(cx, P, l, x_mid, x_next)
        x_cur = x_next
    return cx.nc


_PROGRAM = None
N_CORES = 8


def kernel(**inputs):
    global _PROGRAM
    if _PROGRAM is None:
        _PROGRAM = build_program()
    nc = _PROGRAM
    params = {n: np.ascontiguousarray(np.asarray(inputs[n], dtype=np.float32)) for n in PARAM_NAMES}
    x = np.asarray(inputs["x"], dtype=np.float32)
    c = np.asarray(inputs["c"], dtype=np.float32)
    B = x.shape[0]
    in_maps = []
    for core in range(N_CORES):
        b = core % B
        m = {"x": np.ascontiguousarray(x[b]), "c": np.ascontiguousarray(c[b])}
        m.update(params)
        in_maps.append(m)
    res = run_bass_kernel_spmd(nc, in_maps, core_ids=list(range(N_CORES)))
    out = np.stack([np.asarray(res.results[b]["out"], dtype=np.float32) for b in range(B)], axis=0)
    return out
```

```python
import contextlib
import numpy as np
import concourse.bass as bass
import concourse.mybir as mybir
from concourse.bass_utils import run_bass_kernel_spmd

F32 = mybir.dt.float32
BF16 = mybir.dt.bfloat16
AF = mybir.ActivationFunctionType
ALU = mybir.AluOpType

S = 4096
D = 1024
NTB = S // 128
KB = D // 128
DEPTH = 2
IN_W = 12336
C_Q, C_K, C_V, C_F, C_U, C_Z, C_X, C_DT, C_G = 0, 1024, 2048, 3072, 3088, 4112, 6160, 9232, 9264
ALPHA = (2 * DEPTH) ** 0.25
LN_EPS = 1e-5
RMS_EPS = 1e-6
NEG = -30000.0

ENGS = ("sync", "scalar", "vector", "gpsimd", "tensor")
NDMASEM = 32
NSWSEM = 12


class K:
    def __init__(self, nc):
        self.nc = nc
        self.es = contextlib.ExitStack()
        self.q = {e: [] for e in ENGS}
        self.cnt = {e: 0 for e in ENGS}
        self.sem = {e: self.es.enter_context(nc.semaphore("s_" + e)) for e in ENGS}
        self.dsem = [self.es.enter_context(nc.semaphore("d%d" % i)) for i in range(NDMASEM + NSWSEM)]
        self.dcnt = [0] * (NDMASEM + NSWSEM)
        self.dnext = 0
        self.dnext_sw = 0
        self.last_w = {}
        self.readers = {}
        self.seen = {e: {} for e in ENGS}
        self.uid = 0

    def sb(self, name, shape, dt, es=None):
        self.uid += 1
        return (es or self.es).enter_context(self.nc.sbuf_tensor("%s_%d" % (name, self.uid), list(shape), dt))

    def ps(self, name, shape, dt=F32, es=None):
        self.uid += 1
        return (es or self.es).enter_context(self.nc.psum_tensor("%s_%d" % (name, self.uid), list(shape), dt))

    def _deps(self, eng, reads, writes):
        deps = {}

        def add(tok):
            s, v = tok
            if deps.get(id(s), (None, 0))[1] < v:
                deps[id(s)] = (s, v)

        for k in reads:
            if k in self.last_w:
                add(self.last_w[k])
        for k in writes:
            if k in self.last_w:
                add(self.last_w[k])
            for tok in self.readers.get(k, {}).values():
                add(tok)
        waits = []
        seen = self.seen[eng]
        for sid, (s, v) in deps.items():
            if seen.get(sid, 0) >= v:
                continue
            seen[sid] = v
            waits.append((s, v))
        return waits

    def _commit(self, tok, reads, writes):
        for k in reads:
            self.readers.setdefault(k, {})[id(tok[0])] = tok
        for k in writes:
            self.last_w[k] = tok
            self.readers[k] = {}

    def op(self, eng, fn, reads=(), writes=()):
        waits = self._deps(eng, reads, writes)
        self.cnt[eng] += 1
        tok = (self.sem[eng], self.cnt[eng])
        self.q[eng].append((fn, waits, (self.sem[eng], 1)))
        self._commit(tok, reads, writes)

    def dma(self, eng, fn, reads=(), writes=()):
        waits = self._deps(eng, reads, writes)
        if eng == "gpsimd":
            j = NDMASEM + self.dnext_sw
            self.dnext_sw = (self.dnext_sw + 1) % NSWSEM
        else:
            j = self.dnext
            self.dnext = (self.dnext + 1) % NDMASEM
        s = self.dsem[j]
        if self.dcnt[j] > 0 and self.seen[eng].get(id(s), 0) < self.dcnt[j]:
            waits.append((s, self.dcnt[j]))
            self.seen[eng][id(s)] = self.dcnt[j]
        self.dcnt[j] += 16
        tok = (s, self.dcnt[j])
        self.q[eng].append((fn, waits, (s, 16)))
        self._commit(tok, reads, writes)

    def barrier(self):
        for e in ENGS:
            waits = []
            for f in ENGS:
                if f != e and self.cnt[f] > 0 and self.seen[e].get(id(self.sem[f]), 0) < self.cnt[f]:
                    waits.append((self.sem[f], self.cnt[f]))
                    self.seen[e][id(self.sem[f])] = self.cnt[f]
            for j in range(NDMASEM + NSWSEM):
                if self.dcnt[j] > 0 and self.seen[e].get(id(self.dsem[j]), 0) < self.dcnt[j]:
                    waits.append((self.dsem[j], self.dcnt[j]))
                    self.seen[e][id(self.dsem[j])] = self.dcnt[j]
            if self.cnt[e] > 0 and self.seen[e].get(id(self.sem[e]), 0) < self.cnt[e]:
                waits.append((self.sem[e], self.cnt[e]))
                self.seen[e][id(self.sem[e])] = self.cnt[e]
            if waits:
                self.q[e].append((None, waits, None))
        self.last_w = {}
        self.readers = {}

    def flush(self):
        nc = self.nc
        q = self.q
        if not any(q[e] for e in ENGS):
            return
        with nc.allow_non_contiguous_dma(reason="small strided parameter loads"):
            with nc.Block() as block:
                def mk(ename):
                    def body(e):
                        for fn, waits, inc in q[ename]:
                            for s, v in waits:
                                e.wait_ge(s, v)
                            if fn is not None:
                                fn(e).then_inc(inc[0], inc[1])
                    return body
                block.sync(mk("sync"))
                block.scalar(mk("scalar"))
                block.vector(mk("vector"))
                block.gpsimd(mk("gpsimd"))
                block.tensor(mk("tensor"))
        self.q = {e: [] for e in ENGS}

    @contextlib.contextmanager
    def stage(self):
        es = contextlib.ExitStack()
        try:
            yield es
            self.barrier()
            self.flush()
        finally:
            es.close()


def mm(e, out, lhsT, rhs, start, stop):
    return e.matmul(out, lhsT, rhs, start=start, stop=stop)


def load_bf16(k, dst, src, stg, dkey, skey, eng="gpsimd"):
    k.dma("sync", lambda e: e.dma_start(out=stg, in_=src), writes=[skey])
    k.op(eng, lambda e: e.tensor_copy(out=dst, in_=stg), reads=[skey], writes=[dkey])


class Ctx:
    def __init__(self, ext_in=(), ext_out=()):
        self.nc = bass.Bass("TRN2", target_bir_lowering=False)
        self.k = K(self.nc)
        self.ext_in = set(ext_in)
        self.ext_out = set(ext_out)
        self.dr = {}

    def dram(self, name, shape, dt=F32):
        if name not in self.dr:
            kind = "ExternalInput" if name in self.ext_in else ("ExternalOutput" if name in self.ext_out else "Internal")
            self.dr[name] = self.nc.dram_tensor(name, list(shape), dt, kind=kind).ap()
        return self.dr[name]

    def const(self, name, arr):
        if name not in self.dr:
            self.dr[name] = self.nc.inline_tensor(np.ascontiguousarray(arr), name=name).ap()
        return self.dr[name]


PARAM_SHAPES = {
    "w_in": [DEPTH, D, IN_W], "b_forget": [DEPTH, 16], "w_attn_o": [DEPTH, 1024, D], "w_pool": [DEPTH, 4, 256, 256],
    "pool_scale": [DEPTH, 1024], "conv_w": [DEPTH, 4, 3072], "conv_b": [DEPTH, 3072], "dt_bias": [DEPTH, 32],
    "a_log": [DEPTH, 32], "d_skip": [DEPTH, 32], "ssm_norm_w": [DEPTH, 2048], "w_ssm_o": [DEPTH, 2048, D],
    "w_out": [DEPTH, D, D], "w_ada": [DEPTH, D, 6 * D], "b_ada": [DEPTH, 6 * D], "ln_mix_g": [DEPTH, D],
    "ln_mix_b": [DEPTH, D], "ln_ffn_g": [DEPTH, D], "ln_ffn_b": [DEPTH, D], "w_router": [D, 16], "b_router": [16],
    "w_exp_gate": [DEPTH, 16, D, 512], "w_exp_up": [DEPTH, 16, D, 512], "w_exp_down": [DEPTH, 16, 512, D],
}


def setup_consts(cx):
    k = cx.k
    P = {}
    P["ident"] = k.sb("ident", [128, 128], F32)
    P["identb"] = k.sb("identb", [128, 128], BF16)
    P["ones"] = k.sb("ones", [128, 128], F32)
    idn = cx.const("c_ident", np.eye(128, dtype=np.float32))
    k.dma("sync", lambda e: e.dma_start(out=P["ident"][:], in_=idn), writes=["ident"])
    k.op("vector", lambda e: e.tensor_copy(out=P["identb"][:], in_=P["ident"][:]), reads=["ident"], writes=["identb"])
    k.op("vector", lambda e: e.memset(P["ones"][:], 1.0), writes=["ones"])
    P["modT"] = k.sb("modT", [128, 48], F32)
    P["gbc_m"] = k.sb("gbc_m", [128, D], F32)
    P["gbc_f"] = k.sb("gbc_f", [128, D], F32)
    P["lng"] = k.sb("lng", [128, D], F32)
    P["lnb"] = k.sb("lnb", [128, D], F32)
    P["acol"] = k.sb("acol", [64, 64, 32], F32)
    P["dtcol"] = k.sb("dtcol", [64, 64, 32], F32)
    return P


def stage_mod(cx, P, l):
    k = cx.k
    c = cx.dram("c", [D])
    w_ada = cx.dram("w_ada", PARAM_SHAPES["w_ada"])
    b_ada = cx.dram("b_ada", PARAM_SHAPES["b_ada"])
    with k.stage() as es:
        cT = k.sb("cT", [128, KB], F32, es)
        sg = k.sb("csg", [128, KB], F32, es)
        wt = [k.sb("wada%d" % i, [128, KB, 512], F32, es) for i in range(4)]
        brow = [k.sb("brow%d" % i, [1, 512], F32, es) for i in range(2)]
        row = [k.sb("row%d" % i, [1, 512], F32, es) for i in range(2)]
        prow = [k.ps("prow%d" % i, [1, 512], F32, es) for i in range(2)]
        pT = k.ps("pmodT", [128, 48], F32, es)
        pbc = [k.ps("pbc%d" % i, [128, 512], F32, es) for i in range(2)]
        k.dma("sync", lambda e: e.dma_start(out=cT[:], in_=c.rearrange("(kb p) -> p kb", p=128)), writes=["cT"])
        k.op("scalar", lambda e: e.activation(out=sg[:], in_=cT[:], func=AF.Sigmoid), reads=["cT"], writes=["csg"])
        k.op("vector", lambda e: e.tensor_tensor(out=cT[:], in0=cT[:], in1=sg[:], op=ALU.mult), reads=["cT", "csg"], writes=["cT"])
        for j in range(12):
            b = j % 2
            b4 = j % 4
            k.dma("sync", lambda e, j=j, b=b4: e.dma_start(out=wt[b][:], in_=w_ada[l, :, j * 512:(j + 1) * 512].rearrange("(kb p) n -> p kb n", p=128)),
                  writes=[("wada", b4)])
            k.dma("sync", lambda e, j=j, b=b: e.dma_start(out=brow[b][:], in_=b_ada[l, j * 512:(j + 1) * 512].unsqueeze(0)), writes=[("brow", b)])

            def f(e, b=b, b4=b4):
                for kb in range(KB):
                    ins = mm(e, prow[b][:], cT[:, kb:kb + 1], wt[b4][:, kb, :], kb == 0, kb == KB - 1)
                return ins
            k.op("tensor", f, reads=["cT", ("wada", b4)], writes=[("prow", b)])
            k.op("vector", lambda e, b=b: e.tensor_tensor(out=row[b][:], in0=prow[b][:], in1=brow[b][:], op=ALU.add),
                 reads=[("prow", b), ("brow", b)], writes=[("row", b)])

            def g(e, j=j, b=b):
                for i in range(4):
                    ins = mm(e, pT[:, j * 4 + i:j * 4 + i + 1], row[b][0:1, i * 128:(i + 1) * 128], P["ones"][0:1, 0:1], True, True)
                return ins
            k.op("tensor", g, reads=[("row", b), "ones"], writes=["pmodT"])
            if j in (4, 5, 10, 11):
                dst = P["gbc_m"] if j < 6 else P["gbc_f"]
                dkey = "gbc_m" if j < 6 else "gbc_f"
                half = j % 2
                k.op("tensor", lambda e, b=b: mm(e, pbc[b][:], P["ones"][0:1, 0:128], row[b][0:1, :], True, True),
                     reads=[("row", b), "ones"], writes=[("pbc", b)])
                k.op("vector", lambda e, b=b, dst=dst, half=half: e.tensor_copy(out=dst[:, half * 512:(half + 1) * 512], in_=pbc[b][:]),
                     reads=[("pbc", b)], writes=[dkey])
        k.op("vector", lambda e: e.tensor_copy(out=P["modT"][:], in_=pT[:]), reads=["pmodT"], writes=["modT"])
        k.op("vector", lambda e: e.tensor_scalar_add(out=P["modT"][:, 8:16], in0=P["modT"][:, 8:16], scalar1=1.0), reads=["modT"], writes=["modT"])
        k.op("vector", lambda e: e.tensor_scalar_add(out=P["modT"][:, 32:40], in0=P["modT"][:, 32:40], scalar1=1.0), reads=["modT"], writes=["modT"])


def emit_lnt(cx, P, es, x_src, hT, sh_col, sc_col, per_block=None):
    k = cx.k
    xt = [k.sb("lx%d" % i, [128, D], F32, es) for i in range(4)]
    xn = [k.sb("lxn%d" % i, [128, D], F32, es) for i in range(4)]
    st = [k.sb("lst%d" % i, [128, 2, 6], F32, es) for i in range(4)]
    mv = [k.sb("lmv%d" % i, [128, 4], F32, es) for i in range(4)]
    pst = [k.ps("lps%d" % i, [128, KB, 128], F32, es) for i in range(2)]
    h2f_tiles = [k.sb("h2f%d" % i, [128, KB, 128], F32, es) for i in range(2)] if per_block is not None else None

    def front(tb):
        b = tb % 2
        b4 = tb % 4
        k.dma("sync", lambda e, tb=tb, b=b4: e.dma_start(out=xt[b][:], in_=x_src[tb * 128:(tb + 1) * 128, :]), writes=[("lx", b4)])
        k.op("vector", lambda e, b=b4: e.bn_stats(out=st[b][:, 0, :], in_=xt[b][:, 0:512]), reads=[("lx", b4)], writes=[("lst", b4, 0)])
        k.op("vector", lambda e, b=b4: e.bn_stats(out=st[b][:, 1, :], in_=xt[b][:, 512:1024]), reads=[("lx", b4)], writes=[("lst", b4, 1)])
        k.op("vector", lambda e, b=b4: e.bn_aggr(out=mv[b][:, 0:2], in_=st[b][:]), reads=[("lst", b4, 0), ("lst", b4, 1)], writes=[("lmv", b4)])
        k.op("scalar", lambda e, b=b4: e.activation(out=mv[b][:, 2:3], in_=mv[b][:, 1:2], func=AF.Ln, bias=LN_EPS), reads=[("lmv", b4)], writes=[("lmv", b4)])
        k.op("scalar", lambda e, b=b4: e.activation(out=mv[b][:, 2:3], in_=mv[b][:, 2:3], func=AF.Exp, scale=-0.5), reads=[("lmv", b4)], writes=[("lmv", b4)])
        k.op("vector", lambda e, b=b4: e.tensor_scalar(out=xn[b][:], in0=xt[b][:], scalar1=mv[b][:, 0:1], scalar2=mv[b][:, 2:3], op0=ALU.subtract, op1=ALU.mult),
             reads=[("lx", b4), ("lmv", b4)], writes=[("lxn", b4)])

        def tr(e, b=b, b4=b4):
            for kb in range(KB):
                ins = e.transpose(out=pst[b][:, kb, :], in_=xn[b4][:, kb * 128:(kb + 1) * 128], identity=P["ident"][:])
            return ins
        k.op("tensor", tr, reads=[("lxn", b4), "ident"], writes=[("lps", b)])

    def back(tb):
        b = tb % 2
        b4 = tb % 4
        if per_block is None:
            for kb in range(KB):
                k.op("scalar", lambda e, b=b, kb=kb, tb=tb: e.activation(out=hT[:, kb, tb * 128:(tb + 1) * 128], in_=pst[b][:, kb, :], func=AF.Identity,
                                                                     scale=P["modT"][:, sc_col + kb:sc_col + kb + 1], bias=P["modT"][:, sh_col + kb:sh_col + kb + 1]),
                     reads=[("lps", b), "modT"], writes=[("hT", tb)])
        else:
            h2f = h2f_tiles[b]
            for kb in range(KB):
                k.op("vector", lambda e, b=b, kb=kb, h2f=h2f: e.tensor_scalar(out=h2f[:, kb, :], in0=pst[b][:, kb, :], scalar1=P["modT"][:, sc_col + kb:sc_col + kb + 1],
                                                                       scalar2=P["modT"][:, sh_col + kb:sh_col + kb + 1], op0=ALU.mult, op1=ALU.add),
                     reads=[("lps", b), "modT"], writes=[("h2f", b, kb)])
            k.op("scalar", lambda e, tb=tb, h2f=h2f: e.copy(out=hT[:, :, tb * 128:(tb + 1) * 128], in_=h2f[:]), reads=[("h2f", b, kb) for kb in range(KB)], writes=[("hT", tb)])
            return per_block(tb, [("h2f", b, kb) for kb in range(KB)], h2f)
        return None

    front(0)
    deferred = None
    for tb in range(NTB):
        if tb + 1 < NTB:
            front(tb + 1)
        nxt_def = back(tb)
        if deferred is not None:
            deferred()
        deferred = nxt_def
    if deferred is not None:
        deferred()

def stage_att(cx, P, hT, l):
    k = cx.k
    w_in = cx.dram("w_in", PARAM_SHAPES["w_in"])
    b_forget = cx.dram("b_forget", PARAM_SHAPES["b_forget"])
    attT_d = cx.dram("attT_d", [1024, S], BF16)
    um = np.zeros((128, 4, 512), np.float32)
    for r in range(4):
        um[:, r, :] = ((r * 128 + np.arange(128))[:, None] > np.arange(512)[None, :]).astype(np.float32)
    c_U = cx.const("c_U", um)
    sel = np.zeros((16, 2, 16, 66), np.float32)
    for h in range(16):
        sel[h, 0, h, 64] = 1.0
        sel[h, 1, h, 65] = 1.0
    c_sel = cx.const("c_sel", sel)
    hkeys = [("hT", tb) for tb in range(NTB)]
    with contextlib.ExitStack() as es:
        U = k.sb("U", [128, 4, 512], BF16, es)
        negI = k.sb("negI", [128, 128], BF16, es)
        selT = k.sb("selT", [16, 2, 16, 66], BF16, es)
        Fhi = k.sb("Fhi", [16, S], BF16, es)
        Flo = k.sb("Flo", [16, S], BF16, es)
        nFc = k.sb("nFc", [128, NTB, 16], F32, es)
        es1 = contextlib.ExitStack()
        wf = k.sb("wf", [128, KB, 16], BF16, es1)
        wfs = k.sb("wfs", [128, KB, 16], F32, es1)
        negb = k.sb("negb", [16, 1], F32, es1)
        NegF = k.sb("NegF", [16, S], F32, es1)
        pp = [k.ps("pp%d" % i, [128, 512], F32, es1) for i in range(2)]
        k.dma("gpsimd", lambda e: e.dma_start(out=U[:], in_=c_U), writes=["U"])
        k.dma("gpsimd", lambda e: e.dma_start(out=selT[:], in_=c_sel), writes=["selT"])
        k.op("vector", lambda e: e.tensor_scalar_mul(out=negI[:], in0=P["ident"][:], scalar1=NEG), reads=["ident"], writes=["negI"])
        load_bf16(k, wf[:], w_in[l, :, C_F:C_F + 16].rearrange("(kb p) n -> p kb n", p=128), wfs[:], "wf", "wfs")
        k.dma("sync", lambda e: e.dma_start(out=negb[:], in_=b_forget[l, :].unsqueeze(1)), writes=["negb"])
        k.op("vector", lambda e: e.tensor_scalar_mul(out=negb[:], in0=negb[:], scalar1=-1.0), reads=["negb"], writes=["negb"])
        for tc in range(8):
            b = tc % 2

            def f(e, tc=tc, b=b):
                for kb in range(KB):
                    ins = mm(e, pp[b][0:16, :], wf[:, kb, :], hT[:, kb, tc * 512:(tc + 1) * 512], kb == 0, kb == KB - 1)
                return ins
            k.op("tensor", f, reads=["wf"] + hkeys[tc * 4:tc * 4 + 4], writes=[("pp", b)])
            k.op("scalar", lambda e, tc=tc, b=b: e.activation(out=NegF[:, tc * 512:(tc + 1) * 512], in_=pp[b][0:16, :], func=AF.Exp, scale=-1.0, bias=negb[:, 0:1]),
                 reads=[("pp", b), "negb"], writes=["NegF"])
        k.op("scalar", lambda e: e.activation(out=NegF[:], in_=NegF[:], func=AF.Ln, bias=1.0), reads=["NegF"], writes=["NegF"])
        k.op("vector", lambda e: e.tensor_tensor_scan(out=NegF[:], data0=P["ones"][0:16, 0:1].to_broadcast([16, S]), data1=NegF[:], initial=0.0, op0=ALU.mult, op1=ALU.add),
             reads=["NegF", "ones"], writes=["NegF"])
        k.op("vector", lambda e: e.tensor_scalar_mul(out=Fhi[:], in0=NegF[:], scalar1=-1.0), reads=["NegF"], writes=["Fhi"])
        k.op("vector", lambda e: e.scalar_tensor_tensor(out=Flo[:], in0=NegF[:], scalar=-1.0, in1=Fhi[:], op0=ALU.mult, op1=ALU.subtract),
             reads=["NegF", "Fhi"], writes=["Flo"])

        def trf(e):
            for tb in range(NTB):
                ins = e.transpose(out=pp[0][:, tb * 16:(tb + 1) * 16], in_=NegF[0:16, tb * 128:(tb + 1) * 128], identity=P["ident"][0:16, 0:16])
            return ins
        k.op("tensor", trf, reads=["NegF", "ident"], writes=[("pp", 0)])
        k.op("vector", lambda e: e.tensor_copy(out=nFc[:].rearrange("p a b -> p (a b)"), in_=pp[0][:, 0:512]), reads=[("pp", 0)], writes=["nFc"])
        k.barrier()
        k.flush()
        es1.close()
        wqk = k.sb("wqk", [128, KB, 4, 128], BF16, es)
        wv = k.sb("wv", [128, KB, 256], BF16, es)
        qA = [k.sb("qA%d" % i, [66, S], BF16, es) for i in range(2)]
        kA = [k.sb("kA%d" % i, [66, S], BF16, es) for i in range(2)]
        vA = [k.sb("vA%d" % i, [128, NTB, 128], BF16, es) for i in range(2)]
        wst = k.sb("wst", [128, KB, 256], F32, es)
        pT = [k.sb("pT%d" % i, [128, 512], BF16, es) for i in range(4)]
        rr = k.sb("rr", [64, 512], F32, es)
        ot = [k.sb("ot%d" % i, [128, 512], BF16, es) for i in range(2)]
        ps = [k.ps("ps%d" % i, [128, 512], F32, es) for i in range(3)]
        po = [k.ps("po%d" % i, [128, 512], F32, es) for i in range(2)]
        pp = [k.ps("pp%d" % i, [128, 512], F32, es) for i in range(2)]
        pF = k.ps("pF", [2, 512], F32, es)
        for i in range(2):
            k.op("vector", lambda e, i=i: e.memset(vA[i][:], 1.0), writes=[("vA", i)])
        for i in range(2):
            k.op("vector", lambda e, i=i: e.memset(kA[i][64:66, :], 1.0), writes=[("kA", i)])
        cnt = {"pp": 0}

        def load_group(hg):
            k.dma("sync", lambda e, hg=hg: e.dma_start(out=wst[:], in_=wslice(w_in, l, C_Q + hg * 256, 256)), writes=["wst"])
            k.op("gpsimd", lambda e: e.tensor_copy(out=wqk[:, :, :, 0:64], in_=wst[:].rearrange("p kb (h n) -> p kb h n", n=64)), reads=["wst"], writes=["wqk"])
            k.dma("sync", lambda e, hg=hg: e.dma_start(out=wst[:], in_=wslice(w_in, l, C_K + hg * 256, 256)), reads=[], writes=["wst"])
            k.op("gpsimd", lambda e: e.tensor_copy(out=wqk[:, :, :, 64:128], in_=wst[:].rearrange("p kb (h n) -> p kb h n", n=64)), reads=["wst"], writes=["wqk"])
            load_bf16(k, wv[:], wslice(w_in, l, C_V + hg * 256, 256), wst[:], "wv", "wst")

        def proj_groups(h):
            hh = h % 4
            hb = h % 2
            out = []
            for tc in range(8):
                def gqk(tc=tc):
                    b = cnt["pp"] % 2
                    cnt["pp"] += 1

                    def fqk(e):
                        for kb in range(KB):
                            ins = mm(e, pp[b][:], wqk[:, kb, hh, :], hT[:, kb, tc * 512:(tc + 1) * 512], kb == 0, kb == KB - 1)
                        return ins
                    k.op("tensor", fqk, reads=["wqk"] + hkeys[tc * 4:tc * 4 + 4], writes=[("pp", b)])

                    def ff(e):
                        mm(e, pF[:], selT[:, 0, h, 64:66], Fhi[:, tc * 512:(tc + 1) * 512], True, False)
                        return mm(e, pF[:], selT[:, 1, h, 64:66], Flo[:, tc * 512:(tc + 1) * 512], False, True)
                    k.op("tensor", ff, reads=["selT", "Fhi", "Flo"], writes=["pF"])
                    k.op("vector", lambda e: e.tensor_scalar_mul(out=qA[hb][0:64, tc * 512:(tc + 1) * 512], in0=pp[b][0:64, :], scalar1=0.125),
                         reads=[("pp", b)], writes=[("qA", hb, tc)])
                    k.op("vector", lambda e: e.tensor_copy(out=kA[hb][0:64, tc * 512:(tc + 1) * 512], in_=pp[b][64:128, :]),
                         reads=[("pp", b), ("kA", hb)], writes=[("kAc", hb, tc)])
                    k.op("vector", lambda e: e.tensor_copy(out=qA[hb][64:66, tc * 512:(tc + 1) * 512], in_=pF[:]),
                         reads=["pF"], writes=[("qA2", hb, tc)])
                out.append(gqk)

                def gv(tc=tc):
                    b = cnt["pp"] % 2
                    cnt["pp"] += 1

                    def fv(e):
                        for t4 in range(4):
                            tb = tc * 4 + t4
                            for kb in range(KB):
                                ins = mm(e, pp[b][:, t4 * 64:(t4 + 1) * 64], hT[:, kb, tb * 128:(tb + 1) * 128], wv[:, kb, hh * 64:(hh + 1) * 64], kb == 0, kb == KB - 1)
                        return ins
                    k.op("tensor", fv, reads=["wv"] + hkeys[tc * 4:tc * 4 + 4], writes=[("pp", b)])
                    k.op("vector", lambda e: e.tensor_copy(out=vA[hb][:, tc * 4:(tc + 1) * 4, 64:128], in_=pp[b][:, 0:256].rearrange("p (t n) -> p t n", n=64)),
                         reads=[("pp", b), ("vA", hb)], writes=[("vAc", hb, tc)])
                out.append(gv)
            return out

        def finalize(h, tc):
            ob = tc % 2
            k.op("vector", lambda e: e.reciprocal(out=rr[:], in_=po[ob][0:64, :]), reads=[("po", ob)], writes=["rr"])
            k.op("vector", lambda e: e.tensor_tensor(out=ot[ob][64:128, :], in0=po[ob][64:128, :], in1=rr[:], op=ALU.mult), reads=[("po", ob), "rr"], writes=[("ot", ob)])
            k.dma("gpsimd", lambda e: e.dma_start(out=attT_d[h * 64:(h + 1) * 64, tc * 512:(tc + 1) * 512], in_=ot[ob][64:128, :]),
                  reads=[("ot", ob)], writes=[("attT_d", h, tc)])

        load_group(0)
        for g in proj_groups(0):
            g()
        its = [(tc, sb) for tc in range(8) for sb in range(4 * (tc + 1))]
        import os
        LOOK = 2
        DEFER = int(os.environ.get('ATT_DEFER', '3'))
        NOIL = int(os.environ.get('ATT_NOIL', '0'))
        gi = 0
        for h in range(16):
            hb = h % 2
            hh = h % 4
            nxt = proj_groups(h + 1) if h + 1 < 16 else []
            new_group = (h + 1 < 16) and ((h + 1) % 4 == 0)
            if new_group:
                load_group((h + 1) // 4)
            start_at = 48 if new_group else 12
            if NOIL:
                start_at = 10 ** 9
            step = 4 if new_group else 5
            pending = []
            for i in range(len(its) + LOOK):
                if i < len(its):
                    tc, sb = its[i]
                    sj = gi % 3
                    pj = gi % 4
                    gi += 1
                    diag = sb >= 4 * tc

                    def fs(e, tc=tc, sb=sb, sj=sj, hb=hb, diag=diag):
                        ins = mm(e, ps[sj][:], kA[hb][0:66, sb * 128:(sb + 1) * 128], qA[hb][0:66, tc * 512:(tc + 1) * 512], True, not diag)
                        if diag:
                            ins = mm(e, ps[sj][:], negI[:], U[:, sb - 4 * tc, :], False, True)
                        return ins
                    k.op("tensor", fs, reads=[("kA", hb), ("kAc", hb, sb // 4), ("qA", hb, tc), ("qA2", hb, tc), "negI", "U"], writes=[("ps", sj)])
                    k.op("scalar", lambda e, sb=sb, sj=sj, pj=pj, h=h: e.activation(out=pT[pj][:], in_=ps[sj][:], func=AF.Exp, bias=nFc[:, sb, h:h + 1], scale=1.0),
                         reads=[("ps", sj), "nFc"], writes=[("pT", pj)])
                    its_pj = pj
                    if i == 0:
                        pjs = []
                    pjs.append(pj)
                if i >= LOOK:
                    tc, sb = its[i - LOOK]
                    pj = pjs[i - LOOK]
                    ob = tc % 2
                    nsb = 4 * (tc + 1)
                    k.op("tensor", lambda e, sb=sb, pj=pj, ob=ob, hb=hb, nsb=nsb: mm(e, po[ob][:], vA[hb][:, sb, :], pT[pj][:], sb == 0, sb == nsb - 1),
                         reads=[("vA", hb), ("vAc", hb, sb // 4), ("pT", pj)], writes=[("po", ob)])
                    if sb == nsb - 1:
                        pending.append((i + DEFER, tc))
                if nxt and i >= start_at and (i - start_at) % step == 0:
                    nxt.pop(0)()
                while pending and (pending[0][0] <= i or i == len(its) + LOOK - 1):
                    finalize(h, pending.pop(0)[1])
            while nxt:
                nxt.pop(0)()
        k.barrier()
        k.flush()

def wslice(w_in, l, c0, n):
    return w_in[l, :, c0:c0 + n].rearrange("(kb p) n -> p kb n", p=128)


HKEYS = [("hT", tb) for tb in range(NTB)]


def stage_gates(cx, P, hT, l):
    k = cx.k
    w_in = cx.dram("w_in", PARAM_SHAPES["w_in"])
    gT_d = cx.dram("gT_d", [3072, S], BF16)
    with k.stage() as es:
        wg = [k.sb("wg%d" % i, [128, KB, 128], BF16, es) for i in range(2)]
        wgs = [k.sb("wgs%d" % i, [128, KB, 128], F32, es) for i in range(2)]
        stg = [k.sb("gst%d" % i, [128, S], BF16, es) for i in range(2)]
        pp = [k.ps("gpp%d" % i, [128, 512], F32, es) for i in range(2)]
        n = 0
        for cb in range(24):
            b = cb % 2
            load_bf16(k, wg[b][:], wslice(w_in, l, C_G + cb * 128, 128), wgs[b][:], ("wg", b), ("wgs", b))
            for tc in range(8):
                j = n % 2
                n += 1

                def f(e, b=b, tc=tc, j=j):
                    for kb in range(KB):
                        ins = mm(e, pp[j][:], wg[b][:, kb, :], hT[:, kb, tc * 512:(tc + 1) * 512], kb == 0, kb == KB - 1)
                    return ins
                k.op("tensor", f, reads=[("wg", b)] + HKEYS[tc * 4:tc * 4 + 4], writes=[("gpp", j)])
                k.op("scalar", lambda e, b=b, tc=tc, j=j: e.activation(out=stg[b][:, tc * 512:(tc + 1) * 512], in_=pp[j][:], func=AF.Sigmoid),
                     reads=[("gpp", j)], writes=[("gst", b)])
            k.dma("scalar", lambda e, cb=cb, b=b: e.dma_start(out=gT_d[cb * 128:(cb + 1) * 128, :], in_=stg[b][:]), reads=[("gst", b)], writes=[("gT_d", cb)])


def stage_z(cx, P, hT, l):
    k = cx.k
    w_in = cx.dram("w_in", PARAM_SHAPES["w_in"])
    zs_d = cx.dram("zs_d", [S, 2048], BF16)
    with k.stage() as es:
        wz = [k.sb("wz%d" % i, [128, KB, 512], BF16, es) for i in range(2)]
        wzs = [k.sb("wzs%d" % i, [128, KB, 512], F32, es) for i in range(2)]
        stg = [k.sb("zst%d" % i, [128, 512], BF16, es) for i in range(3)]
        pp = [k.ps("zpp%d" % i, [128, 512], F32, es) for i in range(2)]
        n = 0
        for cc in range(4):
            b = cc % 2
            k.dma("gpsimd", lambda e, cc=cc, b=b: e.dma_start(out=wz[b][:], in_=wslice(w_in, l, C_Z + cc * 512, 512)), writes=[("wz", b)])
            for tb in range(NTB):
                j = n % 2
                i = n % 3
                n += 1

                def f(e, b=b, tb=tb, j=j):
                    for kb in range(KB):
                        ins = mm(e, pp[j][:], hT[:, kb, tb * 128:(tb + 1) * 128], wz[b][:, kb, :], kb == 0, kb == KB - 1)
                    return ins
                k.op("tensor", f, reads=[("wz", b), ("hT", tb)], writes=[("zpp", j)])
                k.op("scalar", lambda e, j=j, i=i: e.activation(out=stg[i][:], in_=pp[j][:], func=AF.Silu), reads=[("zpp", j)], writes=[("zst", i)])
                k.dma("scalar", lambda e, cc=cc, tb=tb, i=i: e.dma_start(out=zs_d[tb * 128:(tb + 1) * 128, cc * 512:(cc + 1) * 512], in_=stg[i][:]),
                      reads=[("zst", i)], writes=[("zs_d", cc, tb)])


def stage_pool(cx, P, hT, l):
    k = cx.k
    w_in = cx.dram("w_in", PARAM_SHAPES["w_in"])
    w_pool = cx.dram("w_pool", PARAM_SHAPES["w_pool"])
    pool_scale = cx.dram("pool_scale", PARAM_SHAPES["pool_scale"])
    ypT_d = cx.dram("ypT_d", [1024, S], BF16)
    c_inv = cx.const("c_invpos", np.broadcast_to(1.0 / np.arange(1, 17, dtype=np.float32), (128, 16)))
    with k.stage() as es:
        wu = [k.sb("wu%d" % i, [128, KB, 128], BF16, es) for i in range(2)]
        wp = k.sb("wp", [128, 2, 256], BF16, es)
        wps = k.sb("wps", [128, 2, 256], F32, es)
        wus = [k.sb("wus%d" % i, [128, KB, 128], F32, es) for i in range(2)]
        psc = k.sb("psc", [128, 8], F32, es)
        invp = k.sb("invp", [128, 16], F32, es)
        A_ = [k.sb("pA%d" % i, [128, S], F32, es) for i in range(2)]
        T = [k.sb("pT%d" % i, [128, S], F32, es) for i in range(2)]
        tmp = k.sb("ptmp", [128, 16], F32, es)
        pooled = k.sb("pooled", [128, 2, S], BF16, es)
        stg = [k.sb("pst%d" % i, [128, S], BF16, es) for i in range(1)]
        pp = [k.ps("ppp%d" % i, [128, 512], F32, es) for i in range(2)]
        k.dma("sync", lambda e: e.dma_start(out=psc[:], in_=pool_scale[l, :].rearrange("(cb p) -> p cb", p=128)), writes=["psc"])
        k.dma("sync", lambda e: e.dma_start(out=invp[:], in_=c_inv), writes=["invp"])
        n = 0
        for g in range(4):
            w = 2 ** (g + 1)
            load_bf16(k, wp[:], w_pool[l, g].rearrange("(cb p) n -> p cb n", p=128), wps[:], "wp", "wps")
            for j in range(2):
                cb = g * 2 + j
                b = cb % 2
                A, akey = A_[b], ("pA", b)
                load_bf16(k, wu[b][:], wslice(w_in, l, C_U + cb * 128, 128), wus[b][:], ("wu", b), ("wus", b))
                for tc in range(8):
                    pj = n % 2
                    n += 1

                    def f(e, b=b, tc=tc, pj=pj):
                        for kb in range(KB):
                            ins = mm(e, pp[pj][:], wu[b][:, kb, :], hT[:, kb, tc * 512:(tc + 1) * 512], kb == 0, kb == KB - 1)
                        return ins
                    k.op("tensor", f, reads=[("wu", b)] + HKEYS[tc * 4:tc * 4 + 4], writes=[("ppp", pj)])
                    k.op("scalar", lambda e, tc=tc, pj=pj, A=A: e.copy(out=A[:, tc * 512:(tc + 1) * 512], in_=pp[pj][:]), reads=[("ppp", pj)], writes=[akey])
                src, skey = A, akey
                sh = 1
                ti = 0
                while sh < w:
                    dst, dkey = T[ti], ("pT", ti)
                    k.op("vector", lambda e, src=src, dst=dst, sh=sh: e.tensor_tensor(out=dst[:, sh:S], in0=src[:, sh:S], in1=src[:, 0:S - sh], op=ALU.add),
                         reads=[skey], writes=[dkey])
                    k.op("vector", lambda e, src=src, dst=dst, sh=sh: e.tensor_copy(out=dst[:, 0:sh], in_=src[:, 0:sh]), reads=[skey, dkey], writes=[dkey])
                    src, skey = dst, dkey
                    ti = 1 - ti
                    sh *= 2
                k.op("vector", lambda e, src=src, j=j, w=w, A=A: e.scalar_tensor_tensor(out=pooled[:, j, :], in0=src[:], scalar=1.0 / w, in1=A[:], op0=ALU.mult, op1=ALU.subtract),
                     reads=[skey, akey], writes=[("pooled", j)])
                k.op("vector", lambda e, src=src, w=w: e.tensor_tensor(out=tmp[:, 0:w], in0=src[:, 0:w], in1=invp[:, 0:w], op=ALU.mult), reads=[skey, "invp"], writes=["ptmp"])
                k.op("vector", lambda e, j=j, w=w, A=A: e.tensor_tensor(out=pooled[:, j, 0:w], in0=tmp[:, 0:w], in1=A[:, 0:w], op=ALU.subtract),
                     reads=["ptmp", akey, ("pooled", j)], writes=[("pooled", j)])
            for j2 in range(2):
                ob = g * 2 + j2
                sb_ = 0
                for tc in range(8):
                    pj = n % 2
                    n += 1

                    def f2(e, j2=j2, tc=tc, pj=pj):
                        for cbk in range(2):
                            ins = mm(e, pp[pj][:], wp[:, cbk, j2 * 128:(j2 + 1) * 128], pooled[:, cbk, tc * 512:(tc + 1) * 512], cbk == 0, cbk == 1)
                        return ins
                    k.op("tensor", f2, reads=["wp", ("pooled", 0), ("pooled", 1)], writes=[("ppp", pj)])
                    k.op("scalar", lambda e, ob=ob, sb_=sb_, tc=tc, pj=pj: e.activation(out=stg[sb_][:, tc * 512:(tc + 1) * 512], in_=pp[pj][:], func=AF.Copy, scale=psc[:, ob:ob + 1]),
                         reads=[("ppp", pj), "psc"], writes=[("pst", sb_)])
                k.dma("scalar", lambda e, ob=ob, sb_=sb_: e.dma_start(out=ypT_d[ob * 128:(ob + 1) * 128, :], in_=stg[sb_][:]), reads=[("pst", sb_)], writes=[("ypT_d", ob)])


def stage_ssdprep(cx, P, hT, l):
    k = cx.k
    w_in = cx.dram("w_in", PARAM_SHAPES["w_in"])
    conv_w = cx.dram("conv_w", PARAM_SHAPES["conv_w"])
    conv_b = cx.dram("conv_b", PARAM_SHAPES["conv_b"])
    dt_bias = cx.dram("dt_bias", PARAM_SHAPES["dt_bias"])
    a_log = cx.dram("a_log", PARAM_SHAPES["a_log"])
    xs_d = cx.dram("xs_d", [S, 2048], BF16)
    Btok_d = cx.dram("Btok_d", [S, 512], BF16)
    BT_d = cx.dram("BT_d", [512, S], BF16)
    CT_d = cx.dram("CT_d", [512, S], BF16)
    acs_d = cx.dram("acs_d", [32, S], F32)
    with k.stage() as es:
        wx = [k.sb("wx%d" % i, [128, KB, 128], BF16, es) for i in range(2)]
        wxs = [k.sb("wxs%d" % i, [128, KB, 128], F32, es) for i in range(2)]
        cw = [k.sb("cw%d" % i, [128, 5], F32, es) for i in range(2)]
        xT = [k.sb("xT%d" % i, [128, S + 4], F32, es) for i in range(2)]
        acc = [k.sb("cacc%d" % i, [128, S], F32, es) for i in range(2)]
        xa = [k.sb("xa%d" % i, [128, S], BF16, es) for i in range(2)]
        tst = [k.sb("tst%d" % i, [128, 8, 128], BF16, es) for i in range(2)]
        pp = [k.ps("spp%d" % i, [128, 512], F32, es) for i in range(2)]
        ptr = [k.ps("sptr%d" % i, [128, 8, 128], BF16, es) for i in range(2)]
        for i in range(2):
            k.op("vector", lambda e, i=i: e.memset(xT[i][:, 0:4], 0.0), writes=[("xT0", i)])
        n = 0
        nt = 0
        cntr = {"n": 0, "nt": 0}

        def sec_a(cb):
            b = cb % 2
            load_bf16(k, wx[b][:], wslice(w_in, l, C_X + cb * 128, 128), wxs[b][:], ("wx", b), ("wxs", b))
            k.dma("sync", lambda e: e.dma_start(out=cw[b][:, 0:4], in_=conv_w[l, :, cb * 128:(cb + 1) * 128].rearrange("j c -> c j")), writes=[("cw", b)])
            k.dma("sync", lambda e: e.dma_start(out=cw[b][:, 4:5], in_=conv_b[l, cb * 128:(cb + 1) * 128].unsqueeze(1)), writes=[("cwb", b)])
            for tc in range(8):
                pj = cntr["n"] % 2
                cntr["n"] += 1

                def f(e, tc=tc, pj=pj):
                    for kb in range(KB):
                        ins = mm(e, pp[pj][:], wx[b][:, kb, :], hT[:, kb, tc * 512:(tc + 1) * 512], kb == 0, kb == KB - 1)
                    return ins
                k.op("tensor", f, reads=[("wx", b)] + HKEYS[tc * 4:tc * 4 + 4], writes=[("spp", pj)])
                k.op("scalar", lambda e, tc=tc, pj=pj: e.copy(out=xT[b][:, 3 + tc * 512:3 + (tc + 1) * 512], in_=pp[pj][:]), reads=[("spp", pj)], writes=[("xT", b)])
            k.op("scalar", lambda e: e.activation(out=acc[b][:], in_=xT[b][:, 0:S], func=AF.Copy, scale=cw[b][:, 0:1]), reads=[("xT", b), ("xT0", b), ("cw", b)], writes=[("cacc", b)])

        def sec_b(cb):
            b = cb % 2
            for j in range(1, 4):
                k.op("vector", lambda e, j=j: e.scalar_tensor_tensor(out=acc[b][:], in0=xT[b][:, j:j + S], scalar=cw[b][:, j:j + 1], in1=acc[b][:], op0=ALU.mult, op1=ALU.add),
                     reads=[("xT", b), ("xT0", b), ("cw", b), ("cacc", b)], writes=[("cacc", b)])

        def sec_c(cb):
            b = cb % 2
            k.op("scalar", lambda e: e.activation(out=xa[b][:], in_=acc[b][:], func=AF.Silu, bias=cw[b][:, 4:5]), reads=[("cacc", b), ("cwb", b)], writes=[("xa", b)])

        def sec_d(cb):
            b = cb % 2
            if cb < 20:
                dst = xs_d[:, cb * 128:(cb + 1) * 128] if cb < 16 else Btok_d[:, (cb - 16) * 128:(cb - 15) * 128]
                for tg in range(4):
                    tj = cntr["nt"] % 2
                    cntr["nt"] += 1

                    def ftr(e, tg=tg, tj=tj):
                        for i in range(8):
                            tb = tg * 8 + i
                            ins = e.transpose(out=ptr[tj][:, i, :], in_=xa[b][:, tb * 128:(tb + 1) * 128], identity=P["identb"][:])
                        return ins
                    k.op("tensor", ftr, reads=[("xa", b), "identb"], writes=[("sptr", tj)])
                    k.op("vector", lambda e, tj=tj: e.tensor_copy(out=tst[tj][:], in_=ptr[tj][:]), reads=[("sptr", tj)], writes=[("tst", tj)])
                    k.dma("scalar", lambda e, tg=tg, tj=tj: e.dma_start(out=dst[tg * 1024:(tg + 1) * 1024, :].rearrange("(i p) c -> p i c", p=128), in_=tst[tj][:]),
                          reads=[("tst", tj)], writes=[("tok_d", cb, tg)])
            if cb >= 16:
                dT = BT_d if cb < 20 else CT_d
                r0 = (cb - 16) * 128 if cb < 20 else (cb - 20) * 128
                k.dma("scalar", lambda e: e.dma_start(out=dT[r0:r0 + 128, :], in_=xa[b][:]), reads=[("xa", b)], writes=[("fm_d", cb)])

        for i in range(26):
            if i < 24:
                sec_a(i)
            if i >= 2:
                sec_d(i - 2)
            if i < 24:
                sec_b(i)
            if 1 <= i <= 24:
                sec_c(i - 1)
    with k.stage() as es:
        wxs = [k.sb("wds", [128, KB, 32], F32, es)]
        wdt = k.sb("wdt", [128, KB, 32], BF16, es)
        dtb = k.sb("dtb", [32, 2], F32, es)
        dtT = k.sb("dtT", [32, S], F32, es)
        Gc = k.sb("Gc", [32, S], F32, es)
        Gprev = k.sb("Gprev", [32, 64], F32, es)
        pp = [k.ps("dpp%d" % i, [128, 512], F32, es) for i in range(2)]
        pc = k.ps("spc", [64, 16, 32], F32, es)
        n = 0
        load_bf16(k, wdt[:], wslice(w_in, l, C_DT, 32), wxs[0][:], "wdt", "wds")
        k.dma("sync", lambda e: e.dma_start(out=dtb[:, 0:1], in_=dt_bias[l, :].unsqueeze(1)), writes=["dtb0"])
        k.dma("sync", lambda e: e.dma_start(out=dtb[:, 1:2], in_=a_log[l, :].unsqueeze(1)), writes=["dtb1"])
        k.op("scalar", lambda e: e.activation(out=dtb[:, 1:2], in_=dtb[:, 1:2], func=AF.Exp), reads=["dtb1"], writes=["dtb1"])
        k.op("vector", lambda e: e.tensor_scalar_mul(out=dtb[:, 1:2], in0=dtb[:, 1:2], scalar1=-1.0), reads=["dtb1"], writes=["dtb1"])
        for tc in range(8):
            pj = n % 2
            n += 1

            def fd(e, tc=tc, pj=pj):
                for kb in range(KB):
                    ins = mm(e, pp[pj][0:32, :], wdt[:, kb, :], hT[:, kb, tc * 512:(tc + 1) * 512], kb == 0, kb == KB - 1)
                return ins
            k.op("tensor", fd, reads=["wdt"] + HKEYS[tc * 4:tc * 4 + 4], writes=[("spp", pj)])
            k.op("scalar", lambda e, tc=tc, pj=pj: e.activation(out=dtT[:, tc * 512:(tc + 1) * 512], in_=pp[pj][0:32, :], func=AF.Exp, bias=dtb[:, 0:1]),
                 reads=[("spp", pj), "dtb0"], writes=["dtT"])
        k.op("scalar", lambda e: e.activation(out=dtT[:], in_=dtT[:], func=AF.Ln, bias=1.0), reads=["dtT"], writes=["dtT"])
        k.op("vector", lambda e: e.tensor_scalar_mul(out=Gc[:], in0=dtT[:], scalar1=dtb[:, 1:2]), reads=["dtT", "dtb1"], writes=["Gc"])
        k.op("vector", lambda e: e.tensor_tensor_scan(out=Gc[:], data0=P["ones"][0:32, 0:1].to_broadcast([32, S]), data1=Gc[:], initial=0.0, op0=ALU.mult, op1=ALU.add),
             reads=["Gc", "ones"], writes=["Gc"])
        k.op("vector", lambda e: e.memset(Gprev[:, 0:1], 0.0), writes=["Gprev0"])
        k.op("vector", lambda e: e.tensor_copy(out=Gprev[:, 1:64], in_=Gc[:].rearrange("p (c l) -> p c l", l=64)[:, 0:63, 63]), reads=["Gc"], writes=["Gprev"])
        acs = Gc
        k.op("vector", lambda e: e.tensor_tensor(out=acs[:].rearrange("p (c l) -> p c l", l=64), in0=Gc[:].rearrange("p (c l) -> p c l", l=64),
                                                 in1=Gprev[:].unsqueeze(2).to_broadcast([32, 64, 64]), op=ALU.subtract),
             reads=["Gc", "Gprev", "Gprev0"], writes=["acs", "Gc"])
        k.dma("sync", lambda e: e.dma_start(out=acs_d, in_=acs[:]), reads=["acs"], writes=["acs_d"])
        for src, skey, dst, dkey in ((acs, "acs", P["acol"], "acol"), (dtT, "dtT", P["dtcol"], "dtcol")):
            for q4 in range(4):
                def ft(e, src=src, q4=q4):
                    for i in range(16):
                        c = q4 * 16 + i
                        ins = e.transpose(out=pc[:, i, :], in_=src[0:32, c * 64:(c + 1) * 64], identity=P["ident"][0:32, 0:32])
                    return ins
                k.op("tensor", ft, reads=[skey, "ident"], writes=["spc"])
                k.op("vector", lambda e, dst=dst, q4=q4: e.tensor_copy(out=dst[:, q4 * 16:(q4 + 1) * 16, :], in_=pc[:]), reads=["spc"], writes=[dkey])


def stage_ssd(cx, P, l):
    k = cx.k
    xs_d = cx.dram("xs_d", [S, 2048], BF16)
    Btok_d = cx.dram("Btok_d", [S, 512], BF16)
    BT_d = cx.dram("BT_d", [512, S], BF16)
    CT_d = cx.dram("CT_d", [512, S], BF16)
    acs_d = cx.dram("acs_d", [32, S], F32)
    zs_d = cx.dram("zs_d", [S, 2048], BF16)
    d_skip = cx.dram("d_skip", PARAM_SHAPES["d_skip"])
    ynT_d = cx.dram("ynT_d", [2048, S], BF16)
    c_tri = cx.const("c_tri", (np.arange(64)[:, None] <= np.arange(64)[None, :]).astype(np.float32))
    acol, dtcol = P["acol"], P["dtcol"]
    with k.stage() as es:
        tri = k.sb("tri", [64, 64], F32, es)
        Dbc = k.sb("Dbc", [64, 32], F32, es)
        DI = k.sb("DI", [64, 32, 64], BF16, es)
        rowb = [k.sb("rowb%d" % i, [128, 32, 64], F32, es) for i in range(2)]
        xin = [k.sb("xin%d" % i, [64, 2048], BF16, es) for i in range(2)]
        btok = [k.sb("btok%d" % i, [64, 512], BF16, es) for i in range(2)]
        bct = [k.sb("bct%d" % i, [128, 8, 64], BF16, es) for i in range(2)]
        zs = [k.sb("zsc%d" % i, [64, 2048], BF16, es) for i in range(2)]
        seg_ = [k.sb("seg", [64, 32, 64], F32, es) for _i in range(2)]
        tmp_ = [k.sb("stmp", [64, 32, 64], F32, es) for _i in range(2)]
        sc_ = [k.sb("scoresT", [64, 32, 64], BF16, es) for _i in range(2)]
        eab_ = [k.sb("eab", [128, 32, 64], F32, es) for _i in range(2)]
        CTe_ = [k.sb("CTe", [128, 32, 64], BF16, es) for _i in range(2)]
        CBm_ = [k.sb("CBm", [64, 4, 64], F32, es) for _i in range(2)]
        dd_ = [k.sb("dd", [64, 32], F32, es) for _i in range(2)]
        cdb_ = [k.sb("cdb", [128, 32], F32, es) for _i in range(2)]
        xsc_ = [k.sb("xsc", [64, 32, 64], BF16, es) for _i in range(2)]
        H = k.sb("H", [128, 32, 64], F32, es)
        Hb = k.sb("Hb", [128, 2048], BF16, es)
        yz_ = [k.sb("yz", [64, 2048], F32, es) for _i in range(2)]
        junk_ = [k.sb("junk", [64, 512], F32, es) for _i in range(2)]
        ss_ = [k.sb("ss", [64, 8], F32, es) for _i in range(2)]
        yn_ = [k.sb("yn", [64, 2048], BF16, es) for _i in range(2)]
        ynst = [k.sb("ynst%d" % i, [128, 16, 256], BF16, es) for i in range(2)]
        pcb_ = [k.ps("pcb%d" % i, [64, 4, 64], F32, es) for i in range(2)]
        py = [k.ps("py%d" % i, [64, 512], F32, es) for i in range(2)]
        pst = [k.ps("pst%d" % i, [128, 512], F32, es) for i in range(2)]
        ptr_ = [k.ps("yptr%d" % i, [128, 16, 64], BF16, es) for i in range(2)]
        seg = tmp = sc = eab = CTe = CBm = dd = cdb = xsc = yz = junk = ss = yn = pcb = None
        k.dma("sync", lambda e, seg=seg, tmp=tmp, sc=sc, eab=eab, CTe=CTe, CBm=CBm, dd=dd, cdb=cdb, xsc=xsc, yz=yz, junk=junk, ss=ss, yn=yn, pcb=pcb: e.dma_start(out=tri[:], in_=c_tri), writes=["tri"])
        k.dma("sync", lambda e, seg=seg, tmp=tmp, sc=sc, eab=eab, CTe=CTe, CBm=CBm, dd=dd, cdb=cdb, xsc=xsc, yz=yz, junk=junk, ss=ss, yn=yn, pcb=pcb: e.dma_start(out=Dbc[:], in_=d_skip[l, :].partition_broadcast(64)), writes=["Dbc"])
        k.op("vector", lambda e, seg=seg, tmp=tmp, sc=sc, eab=eab, CTe=CTe, CBm=CBm, dd=dd, cdb=cdb, xsc=xsc, yz=yz, junk=junk, ss=ss, yn=yn, pcb=pcb: e.tensor_tensor(out=DI[:], in0=P["ident"][0:64, 0:64].unsqueeze(1).to_broadcast([64, 32, 64]),
                                                 in1=Dbc[:].unsqueeze(2).to_broadcast([64, 32, 64]), op=ALU.mult), reads=["ident", "Dbc"], writes=["DI"])
        k.op("vector", lambda e, seg=seg, tmp=tmp, sc=sc, eab=eab, CTe=CTe, CBm=CBm, dd=dd, cdb=cdb, xsc=xsc, yz=yz, junk=junk, ss=ss, yn=yn, pcb=pcb: e.memset(H[:], 0.0), writes=[("H", g) for g in range(4)])
        k.op("vector", lambda e, seg=seg, tmp=tmp, sc=sc, eab=eab, CTe=CTe, CBm=CBm, dd=dd, cdb=cdb, xsc=xsc, yz=yz, junk=junk, ss=ss, yn=yn, pcb=pcb: e.memset(Hb[:], 0.0), writes=[("Hb", g) for g in range(4)])
        def front(c):
                b = c % 2
                t0 = c * 64
                seg, tmp, sc, eab, CTe, CBm, dd, cdb, xsc, yz, junk, ss, yn, pcb = (seg_[b], tmp_[b], sc_[b], eab_[b], CTe_[b], CBm_[b], dd_[b], cdb_[b], xsc_[b], yz_[b], junk_[b], ss_[b], yn_[b], pcb_[b])
                ptr = ptr_[b]
                k.dma("sync", lambda e, seg=seg, tmp=tmp, sc=sc, eab=eab, CTe=CTe, CBm=CBm, dd=dd, cdb=cdb, xsc=xsc, yz=yz, junk=junk, ss=ss, yn=yn, pcb=pcb, b=b, t0=t0: e.dma_start(out=rowb[b][:], in_=acs_d[:, t0:t0 + 64].partition_broadcast(128)), writes=[("rowb", b)])
                k.dma("sync", lambda e, seg=seg, tmp=tmp, sc=sc, eab=eab, CTe=CTe, CBm=CBm, dd=dd, cdb=cdb, xsc=xsc, yz=yz, junk=junk, ss=ss, yn=yn, pcb=pcb, b=b, t0=t0: e.dma_start(out=xin[b][:], in_=xs_d[t0:t0 + 64, :]), writes=[("xin", b)])
                k.dma("sync", lambda e, seg=seg, tmp=tmp, sc=sc, eab=eab, CTe=CTe, CBm=CBm, dd=dd, cdb=cdb, xsc=xsc, yz=yz, junk=junk, ss=ss, yn=yn, pcb=pcb, b=b, t0=t0: e.dma_start(out=btok[b][:], in_=Btok_d[t0:t0 + 64, :]), writes=[("btok", b)])
                k.dma("sync", lambda e, seg=seg, tmp=tmp, sc=sc, eab=eab, CTe=CTe, CBm=CBm, dd=dd, cdb=cdb, xsc=xsc, yz=yz, junk=junk, ss=ss, yn=yn, pcb=pcb, b=b, t0=t0: e.dma_start(out=bct[b][:, 0:4, :], in_=BT_d[:, t0:t0 + 64].rearrange("(g n) t -> n g t", n=128)), writes=[("bctB", b)])
                k.dma("sync", lambda e, seg=seg, tmp=tmp, sc=sc, eab=eab, CTe=CTe, CBm=CBm, dd=dd, cdb=cdb, xsc=xsc, yz=yz, junk=junk, ss=ss, yn=yn, pcb=pcb, b=b, t0=t0: e.dma_start(out=bct[b][:, 4:8, :], in_=CT_d[:, t0:t0 + 64].rearrange("(g n) t -> n g t", n=128)), writes=[("bctC", b)])
                k.dma("sync", lambda e, seg=seg, tmp=tmp, sc=sc, eab=eab, CTe=CTe, CBm=CBm, dd=dd, cdb=cdb, xsc=xsc, yz=yz, junk=junk, ss=ss, yn=yn, pcb=pcb, b=b, t0=t0: e.dma_start(out=zs[b][:], in_=zs_d[t0:t0 + 64, :]), writes=[("zsc", b)])
                k.op("vector", lambda e, seg=seg, tmp=tmp, sc=sc, eab=eab, CTe=CTe, CBm=CBm, dd=dd, cdb=cdb, xsc=xsc, yz=yz, junk=junk, ss=ss, yn=yn, pcb=pcb, b=b, c=c: e.tensor_tensor(out=seg[:], in0=rowb[b][0:64, :, :], in1=acol[:, c, :].unsqueeze(2).to_broadcast([64, 32, 64]), op=ALU.subtract),
                     reads=[("rowb", b), "acol"], writes=[("seg", b)])
                k.op("scalar", lambda e, seg=seg, tmp=tmp, sc=sc, eab=eab, CTe=CTe, CBm=CBm, dd=dd, cdb=cdb, xsc=xsc, yz=yz, junk=junk, ss=ss, yn=yn, pcb=pcb: e.activation(out=seg[:], in_=seg[:], func=AF.Exp), reads=[("seg", b)], writes=[("seg", b)])

                def fcb(e, b=b, pcb=pcb):
                    for g in range(4):
                        ins = mm(e, pcb[:, g, :], bct[b][:, g, :], bct[b][:, 4 + g, :], True, True)
                    return ins
                k.op("tensor", fcb, reads=[("bctB", b), ("bctC", b)], writes=[("pcb", b)])
                k.op("vector", lambda e, seg=seg, tmp=tmp, sc=sc, eab=eab, CTe=CTe, CBm=CBm, dd=dd, cdb=cdb, xsc=xsc, yz=yz, junk=junk, ss=ss, yn=yn, pcb=pcb: e.tensor_tensor(out=CBm[:], in0=pcb[:], in1=tri[:].unsqueeze(1).to_broadcast([64, 4, 64]), op=ALU.mult), reads=[("pcb", b), "tri"], writes=[("CBm", b)])
                for g in range(4):
                    k.op("vector", lambda e, seg=seg, tmp=tmp, sc=sc, eab=eab, CTe=CTe, CBm=CBm, dd=dd, cdb=cdb, xsc=xsc, yz=yz, junk=junk, ss=ss, yn=yn, pcb=pcb, g=g: e.scalar_tensor_tensor(out=tmp[:, g * 8:(g + 1) * 8, :], in0=seg[:, g * 8:(g + 1) * 8, :], scalar=1.0,
                                                                         in1=CBm[:, g, :].unsqueeze(1).to_broadcast([64, 8, 64]), op0=ALU.min, op1=ALU.mult),
                         reads=[("seg", b), ("CBm", b)], writes=[("stmp", b, g)])
                k.op("vector", lambda e, seg=seg, tmp=tmp, sc=sc, eab=eab, CTe=CTe, CBm=CBm, dd=dd, cdb=cdb, xsc=xsc, yz=yz, junk=junk, ss=ss, yn=yn, pcb=pcb, c=c: e.tensor_tensor(out=sc[:], in0=tmp[:], in1=dtcol[:, c, :].unsqueeze(2).to_broadcast([64, 32, 64]), op=ALU.mult),
                     reads=[("stmp", b, g) for g in range(4)] + ["dtcol"], writes=[("scoresT", b)])
                k.op("scalar", lambda e, seg=seg, tmp=tmp, sc=sc, eab=eab, CTe=CTe, CBm=CBm, dd=dd, cdb=cdb, xsc=xsc, yz=yz, junk=junk, ss=ss, yn=yn, pcb=pcb, b=b: e.activation(out=eab[:], in_=rowb[b][:], func=AF.Exp), reads=[("rowb", b)], writes=[("eab", b)])
                for g in range(4):
                    k.op("gpsimd", lambda e, seg=seg, tmp=tmp, sc=sc, eab=eab, CTe=CTe, CBm=CBm, dd=dd, cdb=cdb, xsc=xsc, yz=yz, junk=junk, ss=ss, yn=yn, pcb=pcb, g=g, b=b: e.tensor_tensor(out=CTe[:, g * 8:(g + 1) * 8, :], in0=eab[:, g * 8:(g + 1) * 8, :],
                                                                        in1=bct[b][:, 4 + g, :].unsqueeze(1).to_broadcast([128, 8, 64]), op=ALU.mult),
                         reads=[("eab", b), ("bctC", b)], writes=[("CTe", b, g)])
                k.op("vector", lambda e, seg=seg, tmp=tmp, sc=sc, eab=eab, CTe=CTe, CBm=CBm, dd=dd, cdb=cdb, xsc=xsc, yz=yz, junk=junk, ss=ss, yn=yn, pcb=pcb, b=b, c=c: e.tensor_tensor(out=dd[:], in0=rowb[b][0:64, :, 63], in1=acol[:, c, :], op=ALU.subtract), reads=[("rowb", b), "acol"], writes=[("dd", b)])
                k.op("scalar", lambda e, seg=seg, tmp=tmp, sc=sc, eab=eab, CTe=CTe, CBm=CBm, dd=dd, cdb=cdb, xsc=xsc, yz=yz, junk=junk, ss=ss, yn=yn, pcb=pcb: e.activation(out=dd[:], in_=dd[:], func=AF.Exp), reads=[("dd", b)], writes=[("dd", b)])
                k.op("vector", lambda e, seg=seg, tmp=tmp, sc=sc, eab=eab, CTe=CTe, CBm=CBm, dd=dd, cdb=cdb, xsc=xsc, yz=yz, junk=junk, ss=ss, yn=yn, pcb=pcb, c=c: e.tensor_tensor(out=dd[:], in0=dd[:], in1=dtcol[:, c, :], op=ALU.mult), reads=[("dd", b), "dtcol"], writes=[("dd", b)])
                k.op("gpsimd", lambda e, seg=seg, tmp=tmp, sc=sc, eab=eab, CTe=CTe, CBm=CBm, dd=dd, cdb=cdb, xsc=xsc, yz=yz, junk=junk, ss=ss, yn=yn, pcb=pcb, b=b: e.tensor_tensor(out=xsc[:], in0=xin[b][:].rearrange("p (h n) -> p h n", n=64), in1=dd[:].unsqueeze(2).to_broadcast([64, 32, 64]), op=ALU.mult),
                     reads=[("xin", b), ("dd", b)], writes=[("xsc", b)])
                k.op("scalar", lambda e, seg=seg, tmp=tmp, sc=sc, eab=eab, CTe=CTe, CBm=CBm, dd=dd, cdb=cdb, xsc=xsc, yz=yz, junk=junk, ss=ss, yn=yn, pcb=pcb, b=b: e.activation(out=cdb[:], in_=rowb[b][:, :, 63], func=AF.Exp), reads=[("rowb", b)], writes=[("cdb", b)])

        def back(c):
                b = c % 2
                t0 = c * 64
                seg, tmp, sc, eab, CTe, CBm, dd, cdb, xsc, yz, junk, ss, yn, pcb = (seg_[b], tmp_[b], sc_[b], eab_[b], CTe_[b], CBm_[b], dd_[b], cdb_[b], xsc_[b], yz_[b], junk_[b], ss_[b], yn_[b], pcb_[b])
                ptr = ptr_[b]
                for g in range(4):
                    pj = g % 2

                    def fy(e, g=g, b=b, pj=pj, sc=sc, CTe=CTe):
                        for r in range(8):
                            h = g * 8 + r
                            o = py[pj][:, r * 64:(r + 1) * 64]
                            xh = xin[b][:, h * 64:(h + 1) * 64]
                            mm(e, o, sc[:, h, :], xh, True, False)
                            mm(e, o, CTe[:, h, :], Hb[:, h * 64:(h + 1) * 64], False, False)
                            ins = mm(e, o, DI[:, h, :], xh, False, True)
                        return ins
                    k.op("tensor", fy, reads=[("scoresT", b), ("xin", b), ("CTe", b, g), ("Hb", g), "DI"], writes=[("py", pj)])
                    k.op("tensor", lambda e, seg=seg, tmp=tmp, sc=sc, eab=eab, CTe=CTe, CBm=CBm, dd=dd, cdb=cdb, xsc=xsc, yz=yz, junk=junk, ss=ss, yn=yn, pcb=pcb, g=g, b=b, pj=pj: mm(e, pst[pj][:], btok[b][:, g * 128:(g + 1) * 128], xsc[:, g * 8:(g + 1) * 8, :], True, True),
                         reads=[("btok", b), ("xsc", b)], writes=[("pst", pj)])
                    k.op("vector", lambda e, seg=seg, tmp=tmp, sc=sc, eab=eab, CTe=CTe, CBm=CBm, dd=dd, cdb=cdb, xsc=xsc, yz=yz, junk=junk, ss=ss, yn=yn, pcb=pcb, g=g: e.tensor_tensor(out=H[:, g * 8:(g + 1) * 8, :], in0=H[:, g * 8:(g + 1) * 8, :],
                                                                  in1=cdb[:, g * 8:(g + 1) * 8].unsqueeze(2).to_broadcast([128, 8, 64]), op=ALU.mult),
                         reads=[("H", g), ("cdb", b)], writes=[("H", g)])
                    k.op("vector", lambda e, seg=seg, tmp=tmp, sc=sc, eab=eab, CTe=CTe, CBm=CBm, dd=dd, cdb=cdb, xsc=xsc, yz=yz, junk=junk, ss=ss, yn=yn, pcb=pcb, g=g, pj=pj: e.tensor_tensor(out=H[:, g * 8:(g + 1) * 8, :], in0=H[:, g * 8:(g + 1) * 8, :],
                                                                         in1=pst[pj][:].rearrange("p (h n) -> p h n", n=64), op=ALU.add),
                         reads=[("H", g), ("pst", pj)], writes=[("H", g)])
                    k.op("scalar", lambda e, seg=seg, tmp=tmp, sc=sc, eab=eab, CTe=CTe, CBm=CBm, dd=dd, cdb=cdb, xsc=xsc, yz=yz, junk=junk, ss=ss, yn=yn, pcb=pcb, g=g: e.copy(out=Hb[:, g * 512:(g + 1) * 512], in_=H[:, g * 8:(g + 1) * 8, :]), reads=[("H", g)], writes=[("Hb", g)])
                    k.op("vector", lambda e, seg=seg, tmp=tmp, sc=sc, eab=eab, CTe=CTe, CBm=CBm, dd=dd, cdb=cdb, xsc=xsc, yz=yz, junk=junk, ss=ss, yn=yn, pcb=pcb, g=g, b=b, pj=pj: e.tensor_tensor(out=yz[:, g * 512:(g + 1) * 512], in0=py[pj][:], in1=zs[b][:, g * 512:(g + 1) * 512], op=ALU.mult),
                         reads=[("py", pj), ("zsc", b)], writes=[("yz", b, g)])
                    k.op("scalar", lambda e, seg=seg, tmp=tmp, sc=sc, eab=eab, CTe=CTe, CBm=CBm, dd=dd, cdb=cdb, xsc=xsc, yz=yz, junk=junk, ss=ss, yn=yn, pcb=pcb, g=g: e.activation(out=junk[:], in_=yz[:, g * 512:(g + 1) * 512], func=AF.Square, accum_out=ss[:, g:g + 1]),
                         reads=[("yz", b, g)], writes=[("junk", b), ("ss", b, g)])
                k.op("scalar", lambda e, ss=ss: e.activation(out=ss[:, 4:8], in_=ss[:, 0:4], func=AF.Ln, scale=1.0 / 512, bias=RMS_EPS), reads=[("ss", b, g) for g in range(4)], writes=[("rstd", b)])
                k.op("scalar", lambda e, ss=ss: e.activation(out=ss[:, 4:8], in_=ss[:, 4:8], func=AF.Exp, scale=-0.5), reads=[("rstd", b)], writes=[("rstd", b)])
                for g in range(4):
                    k.op("scalar", lambda e, seg=seg, tmp=tmp, sc=sc, eab=eab, CTe=CTe, CBm=CBm, dd=dd, cdb=cdb, xsc=xsc, yz=yz, junk=junk, ss=ss, yn=yn, pcb=pcb, g=g: e.activation(out=yn[:, g * 512:(g + 1) * 512], in_=yz[:, g * 512:(g + 1) * 512], func=AF.Copy, scale=ss[:, 4 + g:5 + g]),
                         reads=[("yz", b, g), ("rstd", b)], writes=[("yn", b, g)])

                def ftr(e, yn=yn, ptr=ptr):
                    for kb in range(16):
                        ins = e.transpose(out=ptr[:, kb, :], in_=yn[0:64, kb * 128:(kb + 1) * 128], identity=P["identb"][0:64, 0:64])
                    return ins
                k.op("tensor", ftr, reads=[("yn", b, g) for g in range(4)] + ["identb"], writes=[("yptr", b)])
                sbi = (c // 4) % 2
                k.op("scalar", lambda e, seg=seg, tmp=tmp, sc=sc, eab=eab, CTe=CTe, CBm=CBm, dd=dd, cdb=cdb, xsc=xsc, yz=yz, junk=junk, ss=ss, yn=yn, pcb=pcb, c=c, sbi=sbi: e.copy(out=ynst[sbi][:, :, (c % 4) * 64:(c % 4 + 1) * 64], in_=ptr[:]), reads=[("yptr", b)], writes=[("ynst", sbi)])
                if c % 4 == 3:
                    tcn = c // 4
                    k.dma("scalar", lambda e, seg=seg, tmp=tmp, sc=sc, eab=eab, CTe=CTe, CBm=CBm, dd=dd, cdb=cdb, xsc=xsc, yz=yz, junk=junk, ss=ss, yn=yn, pcb=pcb, tcn=tcn, sbi=sbi: e.dma_start(out=ynT_d[:, tcn * 256:(tcn + 1) * 256].rearrange("(kb p) t -> p kb t", p=128), in_=ynst[sbi][:]),
                          reads=[("ynst", sbi)], writes=[("ynT_d", tcn)])

        front(0)
        for c in range(64):
            if c + 1 < 64:
                front(c + 1)
            back(c)


class ResLN:
    def __init__(self, cx, P, es, gbc_key, pool_ok=False):
        k = cx.k
        self.cx, self.P, self.gk = cx, P, gbc_key
        self.eng = "gpsimd" if pool_ok else "vector"
        self.xt = [k.sb("rx%d" % i, [128, D], F32, es) for i in range(2)]
        self.r = [k.sb("rr%d" % i, [128, D], F32, es) for i in range(2)]
        self.st = [k.sb("rst%d" % i, [128, 2, 6], F32, es) for i in range(2)]
        self.mv = [k.sb("rmv%d" % i, [128, 4], F32, es) for i in range(2)]
        self.n = 0

    def load_params(self, g_ap, b_ap):
        k, P = self.cx.k, self.P
        k.dma("sync", lambda e: e.dma_start(out=P["lng"][:], in_=g_ap.partition_broadcast(128)), writes=["lng"])
        k.dma("sync", lambda e: e.dma_start(out=P["lnb"][:], in_=b_ap.partition_broadcast(128)), writes=["lnb"])

    def emit(self, tb, mix_halves, mix_keys, x_src, x_dst):
        k, P = self.cx.k, self.P
        b = self.n % 2
        self.n += 1
        xt, r, st, mv = self.xt[b], self.r[b], self.st[b], self.mv[b]
        gbc = P[self.gk]
        k.dma("sync", lambda e: e.dma_start(out=xt[:], in_=x_src[tb * 128:(tb + 1) * 128, :]), writes=[("rx", b)])
        eng = self.eng
        for hf in range(2):
            k.op(eng, lambda e, hf=hf: e.tensor_tensor(out=r[:, hf * 512:(hf + 1) * 512], in0=mix_halves[hf], in1=gbc[:, hf * 512:(hf + 1) * 512], op=ALU.mult),
                 reads=[mix_keys[hf], self.gk], writes=[("rr", b, hf)])
        if eng == "vector":
            k.op("vector", lambda e: e.scalar_tensor_tensor(out=r[:], in0=xt[:], scalar=ALPHA, in1=r[:], op0=ALU.mult, op1=ALU.add),
                 reads=[("rx", b), ("rr", b, 0), ("rr", b, 1)], writes=[("rr", b)])
        else:
            k.op(eng, lambda e: e.tensor_scalar_mul(out=xt[:], in0=xt[:], scalar1=ALPHA), reads=[("rx", b)], writes=[("rx", b)])
            k.op(eng, lambda e: e.tensor_tensor(out=r[:], in0=r[:], in1=xt[:], op=ALU.add), reads=[("rx", b), ("rr", b, 0), ("rr", b, 1)], writes=[("rr", b)])
        k.op("vector", lambda e: e.bn_stats(out=st[:, 0, :], in_=r[:, 0:512]), reads=[("rr", b)], writes=[("rst", b, 0)])
        k.op("vector", lambda e: e.bn_stats(out=st[:, 1, :], in_=r[:, 512:1024]), reads=[("rr", b)], writes=[("rst", b, 1)])
        k.op("vector", lambda e: e.bn_aggr(out=mv[:, 0:2], in_=st[:]), reads=[("rst", b, 0), ("rst", b, 1)], writes=[("rmv", b)])
        k.op("scalar", lambda e: e.activation(out=mv[:, 2:3], in_=mv[:, 1:2], func=AF.Ln, bias=LN_EPS), reads=[("rmv", b)], writes=[("rmv", b)])
        k.op("scalar", lambda e: e.activation(out=mv[:, 2:3], in_=mv[:, 2:3], func=AF.Exp, scale=-0.5), reads=[("rmv", b)], writes=[("rmv", b)])
        k.op(eng, lambda e: e.tensor_scalar(out=xt[:], in0=r[:], scalar1=mv[:, 0:1], scalar2=mv[:, 2:3], op0=ALU.subtract, op1=ALU.mult),
             reads=[("rr", b), ("rmv", b), ("rx", b)], writes=[("rx", b)])
        k.op("gpsimd", lambda e: e.tensor_tensor(out=xt[:], in0=xt[:], in1=P["lng"][:], op=ALU.mult), reads=[("rx", b), "lng"], writes=[("rx", b)])
        k.op("gpsimd", lambda e: e.tensor_tensor(out=xt[:], in0=xt[:], in1=P["lnb"][:], op=ALU.add), reads=[("rx", b), "lnb"], writes=[("rx", b)])
        k.dma("gpsimd", lambda e: e.dma_start(out=x_dst[tb * 128:(tb + 1) * 128, :], in_=xt[:]), reads=[("rx", b)], writes=[("x_dst", tb)])


def stage_merge(cx, P, l, x_src, x_dst):
    k = cx.k
    attT_d = cx.dram("attT_d", [1024, S], BF16)
    ynT_d = cx.dram("ynT_d", [2048, S], BF16)
    ypT_d = cx.dram("ypT_d", [1024, S], BF16)
    gT_d = cx.dram("gT_d", [3072, S], BF16)
    w_attn_o = cx.dram("w_attn_o", PARAM_SHAPES["w_attn_o"])
    w_ssm_o = cx.dram("w_ssm_o", PARAM_SHAPES["w_ssm_o"])
    w_out = cx.dram("w_out", PARAM_SHAPES["w_out"])
    ssm_norm_w = cx.dram("ssm_norm_w", PARAM_SHAPES["ssm_norm_w"])
    ln_g = cx.dram("ln_mix_g", PARAM_SHAPES["ln_mix_g"])
    ln_b = cx.dram("ln_mix_b", PARAM_SHAPES["ln_mix_b"])
    TC = 256
    with k.stage() as es:
        wao = k.sb("wao", [128, 8, D], BF16, es)
        wso = k.sb("wso", [128, 16, D], BF16, es)
        wout = k.sb("wout", [128, 8, D], BF16, es)
        nw = k.sb("nw", [128, 16], F32, es)
        wstg = [k.sb("wstg%d" % i, [128, D], F32, es) for i in range(4)]
        at = [k.sb("m_at%d" % i, [128, 8, TC], BF16, es) for i in range(2)]
        yT = [k.sb("m_yT%d" % i, [128, 16, TC], BF16, es) for i in range(2)]
        yp = [k.sb("m_yp%d" % i, [128, 8, TC], BF16, es) for i in range(2)]
        gt = [k.sb("m_gt%d" % i, [128, 24, TC], BF16, es) for i in range(2)]
        m1 = [k.sb("m_m1%d" % i, [128, TC], F32, es) for i in range(2)]
        m2 = [k.sb("m_m2%d" % i, [128, TC], F32, es) for i in range(2)]
        m3 = [k.sb("m_m3%d" % i, [128, TC], F32, es) for i in range(2)]
        mg = [k.sb("m_mg%d" % i, [128, 8, TC], BF16, es) for i in range(2)]
        p1 = [k.ps("m_p1%d" % i, [128, TC], F32, es) for i in range(2)]
        p2 = [k.ps("m_p2%d" % i, [128, TC], F32, es) for i in range(2)]
        pm = [k.ps("m_pm%d" % i, [128, 512], F32, es) for i in range(4)]
        rl = ResLN(cx, P, es, "gbc_m")
        rl.load_params(ln_g[l, :], ln_b[l, :])
        for kb in range(8):
            k.dma("gpsimd", lambda e, kb=kb: e.dma_start(out=wao[:, kb, :], in_=w_attn_o[l, kb * 128:(kb + 1) * 128, :]), writes=[("wao", kb)])
        for kb in range(8):
            k.dma("gpsimd", lambda e, kb=kb: e.dma_start(out=wout[:, kb, :], in_=w_out[l, kb * 128:(kb + 1) * 128, :]), writes=[("wout", kb)])
        k.dma("sync", lambda e: e.dma_start(out=nw[:], in_=ssm_norm_w[l, :].rearrange("(kb p) -> p kb", p=128)), writes=["nw"])
        for kb in range(16):
            k.dma("gpsimd", lambda e, kb=kb: e.dma_start(out=wso[:, kb, :], in_=w_ssm_o[l, kb * 128:(kb + 1) * 128, :]), writes=[("wso", kb)])
            k.op("vector", lambda e, kb=kb: e.tensor_scalar_mul(out=wso[:, kb, :], in0=wso[:, kb, :], scalar1=nw[:, kb:kb + 1]), reads=[("wso", kb), "nw"], writes=[("wso", kb)])
        wso_keys = [("wso", kb) for kb in range(16)]
        npm = 0
        for tc in range(S // TC):
            b = tc % 2
            c0 = tc * TC
            k.dma("sync", lambda e, b=b, c0=c0: e.dma_start(out=at[b][:], in_=attT_d[:, c0:c0 + TC].rearrange("(kb p) t -> p kb t", p=128)), writes=[("m_at", b)])
            k.dma("sync", lambda e, b=b, c0=c0: e.dma_start(out=yT[b][:], in_=ynT_d[:, c0:c0 + TC].rearrange("(kb p) t -> p kb t", p=128)), writes=[("m_yT", b)])
            k.dma("sync", lambda e, b=b, c0=c0: e.dma_start(out=yp[b][:], in_=ypT_d[:, c0:c0 + TC].rearrange("(kb p) t -> p kb t", p=128)), writes=[("m_yp", b)])
            k.dma("sync", lambda e, b=b, c0=c0: e.dma_start(out=gt[b][:], in_=gT_d[:, c0:c0 + TC].rearrange("(kb p) t -> p kb t", p=128)), writes=[("m_gt", b)])
            for cb in range(8):
                j = cb % 2

                def f1(e, b=b, cb=cb, j=j):
                    for kb in range(8):
                        ins = mm(e, p1[j][:], wao[:, kb, cb * 128:(cb + 1) * 128], at[b][:, kb, :], kb == 0, kb == 7)
                    return ins
                k.op("tensor", f1, reads=[("wao", kb_) for kb_ in range(8)] + [("m_at", b)], writes=[("m_p1", j)])

                def f2(e, b=b, cb=cb, j=j):
                    for kb in range(16):
                        ins = mm(e, p2[j][:], wso[:, kb, cb * 128:(cb + 1) * 128], yT[b][:, kb, :], kb == 0, kb == 15)
                    return ins
                k.op("tensor", f2, reads=wso_keys + [("m_yT", b)], writes=[("m_p2", j)])
                k.op("vector", lambda e, b=b, cb=cb, j=j: e.tensor_tensor(out=m1[j][:], in0=p1[j][:], in1=gt[b][:, cb, :], op=ALU.mult), reads=[("m_p1", j), ("m_gt", b)], writes=[("m_m1", j)])
                k.op("vector", lambda e, b=b, cb=cb, j=j: e.tensor_tensor(out=m2[j][:], in0=p2[j][:], in1=gt[b][:, 16 + cb, :], op=ALU.mult), reads=[("m_p2", j), ("m_gt", b)], writes=[("m_m2", j)])
                k.op("gpsimd", lambda e, b=b, cb=cb, j=j: e.tensor_tensor(out=m3[j][:], in0=yp[b][:, cb, :], in1=gt[b][:, 8 + cb, :], op=ALU.mult), reads=[("m_yp", b), ("m_gt", b)], writes=[("m_m3", j)])
                k.op("vector", lambda e, j=j: e.tensor_tensor(out=m1[j][:], in0=m1[j][:], in1=m2[j][:], op=ALU.add), reads=[("m_m1", j), ("m_m2", j)], writes=[("m_m1", j)])
                k.op("gpsimd", lambda e, b=b, cb=cb, j=j: e.tensor_tensor(out=mg[b][:, cb, :], in0=m1[j][:], in1=m3[j][:], op=ALU.add), reads=[("m_m1", j), ("m_m3", j)], writes=[("m_mg", b, cb)])
            for t2 in range(TC // 128):
                tb = tc * (TC // 128) + t2
                halves, keys = [], []
                for hf in range(2):
                    pj = npm % 4
                    npm += 1

                    def f3(e, b=b, t2=t2, hf=hf, pj=pj):
                        for kb in range(8):
                            ins = mm(e, pm[pj][:], mg[b][:, kb, t2 * 128:(t2 + 1) * 128], wout[:, kb, hf * 512:(hf + 1) * 512], kb == 0, kb == 7)
                        return ins
                    k.op("tensor", f3, reads=[("wout", kb_) for kb_ in range(8)] + [("m_mg", b, cb) for cb in range(8)], writes=[("m_pm", pj)])
                    halves.append(pm[pj][:])
                    keys.append(("m_pm", pj))
                rl.emit(tb, halves, keys, x_src, x_dst)


def stage_moe(cx, P, l, x_src, x_dst):
    k = cx.k
    w_router = cx.dram("w_router", PARAM_SHAPES["w_router"])
    b_router = cx.dram("b_router", PARAM_SHAPES["b_router"])
    w1 = cx.dram("w_exp_gate", PARAM_SHAPES["w_exp_gate"])
    w3 = cx.dram("w_exp_up", PARAM_SHAPES["w_exp_up"])
    w2 = cx.dram("w_exp_down", PARAM_SHAPES["w_exp_down"])
    ln_g = cx.dram("ln_ffn_g", PARAM_SHAPES["ln_ffn_g"])
    ln_b = cx.dram("ln_ffn_b", PARAM_SHAPES["ln_ffn_b"])
    with contextlib.ExitStack() as eso:
        hT = k.sb("h2T", [128, KB, S], BF16, eso)
        wd = k.sb("wd", [128, NTB, 16], F32, eso)
        with k.stage() as es:
            wr = k.sb("wr", [128, KB, 16], F32, es)
            brb = k.sb("brb", [128, 16], F32, es)
            pl = [k.ps("pl%d" % i, [128, 16], F32, es) for i in range(2)]
            T = {n: [k.sb("rt_%s%d" % (n, i), shp, F32, es) for i in range(2)] for n, shp in
                 (("e", [128, 16]), ("pr", [128, 16]), ("sel", [128, 16]), ("ge", [128, 16]), ("sel2", [128, 16]), ("pw", [128, 16]), ("s", [128, 16]))}
            k.dma("sync", lambda e: e.dma_start(out=wr[:], in_=w_router.rearrange("(kb p) n -> p kb n", p=128)), writes=["wr"])
            k.dma("sync", lambda e: e.dma_start(out=brb[:], in_=b_router.partition_broadcast(128)), writes=["brb"])

            def router(tb, hkeys, h2f):
                b = tb % 2
                e_, pr, sel, ge, sel2, pw, s_ = (T[n][b] for n in ("e", "pr", "sel", "ge", "sel2", "pw", "s"))
                kk = ("rt", b)

                def f(e):
                    for kb in range(KB):
                        ins = mm(e, pl[b][:], h2f[:, kb, :], wr[:, kb, :], kb == 0, kb == KB - 1)
                    return ins
                k.op("tensor", f, reads=hkeys + ["wr"], writes=[("pl", b)])
                return lambda: router_dve(tb, b, e_, pr, sel, ge, sel2, pw, s_, kk)

            def router_dve(tb, b, e_, pr, sel, ge, sel2, pw, s_, kk):
                V = lambda fn, r=(), w=(): k.op("vector", fn, reads=list(r) + [kk], writes=list(w) + [kk])
                V(lambda e: e.tensor_reduce(out=s_[:, 0:1], in_=pl[b][:], axis=mybir.AxisListType.X, op=ALU.max), r=[("pl", b)])
                V(lambda e: e.tensor_scalar_mul(out=s_[:, 1:2], in0=s_[:, 0:1], scalar1=-1.0))
                k.op("scalar", lambda e: e.activation(out=e_[:], in_=pl[b][:], func=AF.Exp, bias=s_[:, 1:2], accum_out=s_[:, 2:3]), reads=[("pl", b), kk], writes=[kk])
                V(lambda e: e.reciprocal(out=s_[:, 3:4], in_=s_[:, 2:3]))
                V(lambda e: e.tensor_scalar_mul(out=pr[:], in0=e_[:], scalar1=s_[:, 3:4]))
                V(lambda e: e.tensor_tensor(out=sel[:], in0=pr[:], in1=brb[:], op=ALU.add), r=["brb"])
                s3 = sel[:].rearrange("p (g j) -> p g j", j=4)
                V(lambda e: e.tensor_reduce(out=s_[:, 4:8], in_=s3, axis=mybir.AxisListType.X, op=ALU.max))
                V(lambda e: e.tensor_tensor(out=ge[:].rearrange("p (g j) -> p g j", j=4), in0=s3, in1=s_[:, 4:8].unsqueeze(2).to_broadcast([128, 4, 4]), op=ALU.is_ge))
                V(lambda e: e.scalar_tensor_tensor(out=sel2[:], in0=ge[:], scalar=-1e9, in1=sel[:], op0=ALU.mult, op1=ALU.add))
                V(lambda e: e.tensor_reduce(out=s_[:, 8:12], in_=sel2[:].rearrange("p (g j) -> p g j", j=4), axis=mybir.AxisListType.X, op=ALU.max))
                V(lambda e: e.tensor_tensor(out=s_[:, 4:8], in0=s_[:, 4:8], in1=s_[:, 8:12], op=ALU.add))
                V(lambda e: e.tensor_reduce(out=s_[:, 12:13], in_=s_[:, 4:8], axis=mybir.AxisListType.X, op=ALU.max))
                V(lambda e: e.tensor_scalar(out=s_[:, 4:8], in0=s_[:, 4:8], scalar1=s_[:, 12:13], scalar2=None, op0=ALU.is_ge))
                V(lambda e: e.tensor_tensor(out=ge[:].rearrange("p (g j) -> p g j", j=4), in0=s3, in1=s_[:, 8:12].unsqueeze(2).to_broadcast([128, 4, 4]), op=ALU.is_ge))
                V(lambda e: e.tensor_tensor(out=ge[:].rearrange("p (g j) -> p g j", j=4), in0=ge[:].rearrange("p (g j) -> p g j", j=4),
                                            in1=s_[:, 4:8].unsqueeze(2).to_broadcast([128, 4, 4]), op=ALU.mult))
                V(lambda e: e.tensor_tensor(out=pw[:], in0=pr[:], in1=ge[:], op=ALU.mult))
                V(lambda e: e.tensor_reduce(out=s_[:, 13:14], in_=pw[:], axis=mybir.AxisListType.X, op=ALU.add))
                V(lambda e: e.reciprocal(out=s_[:, 13:14], in_=s_[:, 13:14]))
                V(lambda e: e.tensor_scalar_mul(out=wd[:, tb, :], in0=pw[:], scalar1=s_[:, 13:14]), w=[("wd", tb)])
            emit_lnt(cx, P, es, x_src, hT, 24, 32, per_block=router)
        TCH = 1024
        NB = TCH // 128
        with k.stage() as es:
            rl = ResLN(cx, P, es, "gbc_f")
            rl.load_params(ln_g[l, :], ln_b[l, :])
            W1 = [k.sb("W1_%d" % i, [128, KB, 512], BF16, es) for i in range(2)]
            W3 = [k.sb("W3_%d" % i, [128, KB, 512], BF16, es) for i in range(2)]
            W2 = [k.sb("W2_%d" % i, [128, 4, D], BF16, es) for i in range(2)]
            acc = k.sb("acc", [128, NB, D], F32, es)
            aT = [k.sb("aT%d" % i, [128, 4, 512], BF16, es) for i in range(2)]
            sg = [k.sb("sg%d" % i, [128, 512], BF16, es) for i in range(2)]
            pg = [k.ps("pg%d" % i, [128, 512], F32, es) for i in range(2)]
            pu = [k.ps("pu%d" % i, [128, 512], F32, es) for i in range(2)]
            po = [k.ps("po%d" % i, [128, 512], F32, es) for i in range(2)]
            n = 0
            na = 0
            no = 0
            ne = 0
            pend_ln = []
            for tp in range(S // TCH):
                for ex in range(16):
                    wb = ne % 2
                    ne += 1
                    k.dma("gpsimd", lambda e, ex=ex, wb=wb: e.dma_start(out=W1[wb][:], in_=w1[l, ex].rearrange("(kb p) n -> p kb n", p=128)), writes=[("W1", wb)])
                    k.dma("gpsimd", lambda e, ex=ex, wb=wb: e.dma_start(out=W3[wb][:], in_=w3[l, ex].rearrange("(kb p) n -> p kb n", p=128)), writes=[("W3", wb)])
                    k.dma("gpsimd", lambda e, ex=ex, wb=wb: e.dma_start(out=W2[wb][:], in_=w2[l, ex].rearrange("(fb p) n -> p fb n", p=128)), writes=[("W2", wb)])
                    for sub in range(TCH // 512):
                        t0 = tp * TCH + sub * 512
                        ab = na % 2
                        na += 1
                        hk = HKEYS[t0 // 128:t0 // 128 + 4]
                        for fb in range(4):
                            j = n % 2
                            n += 1
                            if pend_ln:
                                tb_, tbl_ = pend_ln.pop(0)
                                rl.emit(tb_, [acc[:, tbl_, 0:512], acc[:, tbl_, 512:1024]], [("acc", tbl_, 0), ("acc", tbl_, 1)], x_src, x_dst)

                            def fg(e, wb=wb, fb=fb, j=j, t0=t0):
                                for kb in range(KB):
                                    ins = mm(e, pg[j][:], W1[wb][:, kb, fb * 128:(fb + 1) * 128], hT[:, kb, t0:t0 + 512], kb == 0, kb == KB - 1)
                                return ins
                            k.op("tensor", fg, reads=[("W1", wb)] + hk, writes=[("pg", j)])

                            def fu(e, wb=wb, fb=fb, j=j, t0=t0):
                                for kb in range(KB):
                                    ins = mm(e, pu[j][:], W3[wb][:, kb, fb * 128:(fb + 1) * 128], hT[:, kb, t0:t0 + 512], kb == 0, kb == KB - 1)
                                return ins
                            k.op("tensor", fu, reads=[("W3", wb)] + hk, writes=[("pu", j)])
                            k.op("scalar", lambda e, j=j: e.activation(out=sg[j][:], in_=pg[j][:], func=AF.Silu), reads=[("pg", j)], writes=[("sg", j)])
                            k.op("vector", lambda e, j=j, ab=ab, fb=fb: e.tensor_tensor(out=aT[ab][:, fb, :], in0=sg[j][:], in1=pu[j][:], op=ALU.mult),
                                 reads=[("sg", j), ("pu", j)], writes=[("aT", ab, fb)])
                        for t4 in range(4):
                            tbl = sub * 4 + t4
                            tb = t0 // 128 + t4
                            for hf in range(2):
                                oj = no % 2
                                no += 1

                                def fo(e, wb=wb, ab=ab, t4=t4, hf=hf, oj=oj):
                                    for fb in range(4):
                                        ins = mm(e, po[oj][:], aT[ab][:, fb, t4 * 128:(t4 + 1) * 128], W2[wb][:, fb, hf * 512:(hf + 1) * 512], fb == 0, fb == 3)
                                    return ins
                                k.op("tensor", fo, reads=[("W2", wb)] + [("aT", ab, fb) for fb in range(4)], writes=[("po", oj)])
                                dst = acc[:, tbl, hf * 512:(hf + 1) * 512]
                                if ex == 0:
                                    k.op("vector", lambda e, oj=oj, dst=dst, tb=tb, ex=ex: e.tensor_scalar_mul(out=dst, in0=po[oj][:], scalar1=wd[:, tb, ex:ex + 1]),
                                         reads=[("po", oj), ("wd", tb)], writes=[("acc", tbl, hf)])
                                else:
                                    k.op("vector", lambda e, oj=oj, dst=dst, tb=tb, ex=ex: e.scalar_tensor_tensor(out=dst, in0=po[oj][:], scalar=wd[:, tb, ex:ex + 1], in1=dst,
                                                                                                                  op0=ALU.mult, op1=ALU.add),
                                         reads=[("po", oj), ("wd", tb), ("acc", tbl, hf)], writes=[("acc", tbl, hf)])
                        if ex == 15:
                            for t4 in range(4):
                                tbl = sub * 4 + t4
                                pend_ln.append((tp * NB + tbl, tbl))
            for tb_, tbl_ in pend_ln:
                rl.emit(tb_, [acc[:, tbl_, 0:512], acc[:, tbl_, 512:1024]], [("acc", tbl_, 0), ("acc", tbl_, 1)], x_src, x_dst)


PARAM_NAMES = list(PARAM_SHAPES.keys())


def build_program():
    cx = Ctx(ext_in=["x", "c"] + PARAM_NAMES, ext_out=["out"])
    k = cx.k
    for n in PARAM_NAMES:
        cx.dram(n, PARAM_SHAPES[n])
    x_cur = cx.dram("x", [S, D])
    cx.dram("c", [D])
    P = setup_consts(cx)
    for l in range(DEPTH):
        stage_mod(cx, P, l)
        with contextlib.ExitStack() as eso:
            hT = k.sb("hT", [128, KB, S], BF16, eso)
            with k.stage() as es:
                emit_lnt(cx, P, es, x_cur, hT, 0, 8)
            stage_att(cx, P, hT, l)
            stage_gates(cx, P, hT, l)
            stage_z(cx, P, hT, l)
            stage_pool(cx, P, hT, l)
            stage_ssdprep(cx, P, hT, l)
        stage_ssd(cx, P, l)
        x_mid = cx.dram("x_mid", [S, D])
        stage_merge(cx, P, l, x_cur, x_mid)
        x_next = cx.dram("out" if l == DEPTH - 1 else "x_l%d" % l, [S, D])
        stage_moe(cx, P, l, x_mid, x_next)
        x_cur = x_next
    return cx.nc


_PROGRAM = None
N_CORES = 8


def kernel(**inputs):
    global _PROGRAM
    if _PROGRAM is None:
        _PROGRAM = build_program()
    nc = _PROGRAM
    params = {n: np.ascontiguousarray(np.asarray(inputs[n], dtype=np.float32)) for n in PARAM_NAMES}
    x = np.asarray(inputs["x"], dtype=np.float32)
    c = np.asarray(inputs["c"], dtype=np.float32)
    B = x.shape[0]
    in_maps = []
    for core in range(N_CORES):
        b = core % B
        m = {"x": np.ascontiguousarray(x[b]), "c": np.ascontiguousarray(c[b])}
        m.update(params)
        in_maps.append(m)
    res = run_bass_kernel_spmd(nc, in_maps, core_ids=list(range(N_CORES)))
    out = np.stack([np.asarray(res.results[b]["out"], dtype=np.float32) for b in range(B)], axis=0)
    return out
```
